# Optimizing a Trainium2 kernel written in Bass

```python
import math
import jax
import jax.numpy as jnp
from jax import lax
import numpy as np

D_MODEL = 1024
BATCH = 2
SEQ = 8192
DEPTH = 2
DEC_BATCH = 32
DEC_SEQ = 1
PAST_LEN = 8192
PAGE_SIZE = 128

EPS = 1e-6
N_MOD = 6
GDN_HEADS = 4
GDN_DK = 128
GDN_DV = 128
GDN_CONV = 4
GDN_CHUNK = 64
GDN_CONV_CH = GDN_HEADS * (2 * GDN_DK + GDN_DV)
NSA_HEADS = 8
NSA_KV_GROUPS = 2
NSA_HPG = NSA_HEADS // NSA_KV_GROUPS
NSA_DH = 64
NSA_BLOCK = 64
NSA_TOPN = 16
NSA_LOCAL = 2
NSA_WINDOW = 512
NSA_QBLOCK = 128
NSA_FORCE = 1.0e4
REL_BUCKETS = 32
REL_MAX_DIST = 2048
RNN_WIDTH = D_MODEL
RNN_BLOCKS = 8
RNN_BW = RNN_WIDTH // RNN_BLOCKS
RNN_CONV = 4
RG_C = 8.0
FFN_DIM = 2816
N_EXPERTS = 8
TOP_K = 2
EXPERT_DIM = 3584
MOE_BLOCK = 128
P0_SIZES = (GDN_CONV_CH, GDN_HEADS * GDN_DV, GDN_HEADS, GDN_HEADS, NSA_HEADS * NSA_DH, 6 * NSA_KV_GROUPS * NSA_DH, 3 * NSA_HEADS)
P0_WIDTH = sum(P0_SIZES)
MIX_WIDTH = GDN_HEADS * GDN_DV + NSA_HEADS * NSA_DH

kernel_name = 'hybrid_gdn_nsa_rglru_moe_step'


def _split_last(a, sizes):
    out, start = [], 0
    for n in sizes:
        out.append(a[..., start:start + n])
        start += n
    return out


def rms_norm(x, w):
    xf = x.astype(jnp.float32)
    return xf * lax.rsqrt(jnp.mean(xf * xf, axis=-1, keepdims=True) + EPS) * w.astype(jnp.float32)


def _l2norm(a):
    return a * lax.rsqrt(jnp.sum(a * a, axis=-1, keepdims=True) + EPS)


def adaln_params(c, w, b):
    m = jax.nn.silu(c.astype(jnp.float32)) @ w.astype(jnp.float32) + b.astype(jnp.float32)
    m = m.reshape(c.shape[0], N_MOD, 1, D_MODEL)
    return [m[:, i] for i in range(N_MOD)]


def modulate(x, gain, shift, scale):
    return (rms_norm(x, gain) * (1.0 + scale) + shift).astype(x.dtype)


def causal_conv(x, buf, w, b=None):
    t = x.shape[1]
    xc = jnp.concatenate([buf.astype(x.dtype), x], axis=1)
    y = xc[:, 0:t] * w[0]
    for j in range(1, w.shape[0]):
        y = y + xc[:, j:j + t] * w[j]
    if b is not None:
        y = y + b
    return y, xc[:, t:]


def swiglu(h, wg, wu, wd):
    return (jax.nn.silu(h @ wg) * (h @ wu)) @ wd


def gdn_core(q, k, v, log_a, beta, s0):
    bsz, t, nh, _ = q.shape
    dv = v.shape[-1]
    c = GDN_CHUNK
    tp = -(-t // c) * c

    def chunked(a):
        a = jnp.pad(a, [(0, 0), (0, tp - t)] + [(0, 0)] * (a.ndim - 2))
        a = a.reshape((bsz, tp // c, c) + a.shape[2:])
        return jnp.swapaxes(a, 2, 3)

    qc, kc, vc, la, bt = (chunked(a) for a in (q, k, v, log_a, beta))
    g = jnp.cumsum(la, axis=-1)
    incl = jnp.tril(jnp.ones((c, c), bool))
    strict = jnp.tril(jnp.ones((c, c), bool), -1)
    gam = jnp.where(incl, jnp.exp(jnp.where(incl, g[..., :, None] - g[..., None, :], 0.0)), 0.0)
    kk = jnp.einsum('bnhid,bnhjd->bnhij', kc, kc)
    m = jnp.where(strict, bt[..., :, None] * gam * kk, 0.0) + jnp.eye(c, dtype=jnp.float32)
    rhs = jnp.concatenate([bt[..., None] * vc, (bt * jnp.exp(g))[..., None] * kc], axis=-1)
    sol = lax.linalg.triangular_solve(m, rhs, left_side=True, lower=True, unit_diagonal=True)
    vb, w = sol[..., :dv], sol[..., dv:]
    aqk = jnp.einsum('bnhid,bnhjd->bnhij', qc, kc) * gam
    qg = qc * jnp.exp(g)[..., None]
    kd = kc * jnp.exp(g[..., -1:] - g)[..., None]
    gc = jnp.exp(g[..., -1])

    def step(s, xs):
        vb_n, w_n, aqk_n, qg_n, kd_n, gc_n = xs
        u = vb_n - jnp.einsum('bhcd,bhde->bhce', w_n, s)
        o = jnp.einsum('bhcd,bhde->bhce', qg_n, s) + jnp.einsum('bhij,bhje->bhie', aqk_n, u)
        s = gc_n[..., None, None] * s + jnp.einsum('bhcd,bhce->bhde', kd_n, u)
        return s, o

    xs = tuple(jnp.moveaxis(a, 1, 0) for a in (vb, w, aqk, qg, kd, gc))
    s_fin, o = lax.scan(step, s0, xs)
    o = jnp.transpose(o, (1, 0, 3, 2, 4)).reshape(bsz, tp, nh, dv)[:, :t]
    return o, s_fin


def gdn_mixer(qkv_raw, z, a_raw, b_raw, conv_buf, s0, conv_w, a_log, dt_bias, norm_w):
    f32 = jnp.float32
    bsz, t, _ = qkv_raw.shape
    y, new_buf = causal_conv(qkv_raw, conv_buf, conv_w)
    y = jax.nn.silu(y.astype(f32))
    q, k, v = _split_last(y, (GDN_HEADS * GDN_DK, GDN_HEADS * GDN_DK, GDN_HEADS * GDN_DV))
    q = _l2norm(q.reshape(bsz, t, GDN_HEADS, GDN_DK)) * GDN_DK ** -0.5
    k = _l2norm(k.reshape(bsz, t, GDN_HEADS, GDN_DK))
    v = v.reshape(bsz, t, GDN_HEADS, GDN_DV)
    log_a = -jnp.exp(a_log.astype(f32)) * jax.nn.softplus(a_raw.astype(f32) + dt_bias.astype(f32))
    beta = jax.nn.sigmoid(b_raw.astype(f32))
    o, s_fin = gdn_core(q, k, v, log_a, beta, s0.astype(f32))
    o = o * lax.rsqrt(jnp.mean(o * o, axis=-1, keepdims=True) + EPS) * norm_w.astype(f32)
    o = o * jax.nn.silu(z.astype(f32).reshape(bsz, t, GDN_HEADS, GDN_DV))
    return o.reshape(bsz, t, GDN_HEADS * GDN_DV), new_buf, s_fin


def t5_bucket(dist):
    n = jnp.maximum(dist, 0)
    exact = REL_BUCKETS // 2
    nf = jnp.maximum(n, exact).astype(jnp.float32)
    large = exact + (jnp.log(nf / exact) / math.log(REL_MAX_DIST / exact) * (REL_BUCKETS - exact)).astype(jnp.int32)
    return jnp.where(n < exact, n, jnp.minimum(large, REL_BUCKETS - 1))


def head_bias(rel_bias, bucket):
    tq, nk = bucket.shape
    b = rel_bias.astype(jnp.float32)[bucket].reshape(tq, nk, NSA_KV_GROUPS, NSA_HPG)
    return jnp.transpose(b, (0, 2, 3, 1))


def masked_softmax(s, mask):
    s = jnp.where(mask, s.astype(jnp.float32), -1e30)
    m = jnp.max(s, axis=-1, keepdims=True)
    p = jnp.where(mask, jnp.exp(s - m), 0.0)
    return p / jnp.maximum(jnp.sum(p, axis=-1, keepdims=True), 1e-30)


def nsa_compress_and_block(kv4):
    bsz, length = kv4.shape[:2]
    lp = -(-length // NSA_BLOCK) * NSA_BLOCK
    kv4 = jnp.pad(kv4, ((0, 0), (0, lp - length), (0, 0), (0, 0), (0, 0)))
    blocks = kv4.reshape(bsz, lp // NSA_BLOCK, NSA_BLOCK, 4, NSA_KV_GROUPS, NSA_DH)
    means = jnp.mean(blocks[:, :, :, :2], axis=2)
    sel = jnp.transpose(blocks[:, :, :, 2:], (0, 3, 4, 1, 2, 5))
    return means[:, :, 0], means[:, :, 1], sel[:, 0], sel[:, 1]


def nsa_block(q, gates, qpos, kc, vc, kb, vb, kw, vw, kwpos, rel_bias):
    bsz, tq = q.shape[:2]
    nblk = kc.shape[1]
    qg = q.reshape(bsz, tq, NSA_KV_GROUPS, NSA_HPG, NSA_DH)
    cend = (jnp.arange(nblk) + 1) * NSA_BLOCK - 1
    dist_c = qpos[:, None] - cend[None, :]
    s_c = jnp.einsum('bqghd,bngd->bqghn', qg, kc) + head_bias(rel_bias, t5_bucket(dist_c))[None]
    p_c = masked_softmax(s_c, (dist_c >= 0)[None, :, None, None, :])
    o_c = jnp.einsum('bqghn,bngd->bqghd', p_c, vc)
    blk = jnp.arange(nblk)[None, :]
    cur = (qpos // NSA_BLOCK)[:, None]
    valid = blk <= cur
    forced = valid & ((blk == 0) | (blk > cur - NSA_LOCAL))
    imp = jnp.where(forced[None, :, None, :], NSA_FORCE,
                    jnp.where(valid[None, :, None, :], jnp.sum(p_c, axis=3), -1.0))
    n_sel = min(NSA_TOPN, nblk)
    _, sel = lax.top_k(imp, n_sel)
    bi = jnp.arange(bsz)[:, None, None, None]
    gi = jnp.arange(NSA_KV_GROUPS)[None, None, :, None]
    nk = n_sel * NSA_BLOCK
    ks = kb[bi, gi, sel].reshape(bsz, tq, NSA_KV_GROUPS, nk, NSA_DH)
    vs = vb[bi, gi, sel].reshape(bsz, tq, NSA_KV_GROUPS, nk, NSA_DH)
    kpos_s = (sel[..., None] * NSA_BLOCK + jnp.arange(NSA_BLOCK)).reshape(bsz, tq, NSA_KV_GROUPS, nk)
    dist_s = qpos[None, :, None, None] - kpos_s
    bias_s = rel_bias.astype(jnp.float32).reshape(REL_BUCKETS, NSA_KV_GROUPS, NSA_HPG)[t5_bucket(dist_s), gi]
    s_s = jnp.einsum('bqghd,bqgkd->bqghk', qg, ks) + jnp.moveaxis(bias_s, -1, 3)
    p_s = masked_softmax(s_s, (dist_s >= 0)[:, :, :, None, :])
    o_s = jnp.einsum('bqghk,bqgkd->bqghd', p_s, vs)
    dist_w = qpos[:, None] - kwpos[None, :]
    mask_w = (kwpos[None, :] >= 0) & (dist_w >= 0) & (dist_w < NSA_WINDOW)
    s_w = jnp.einsum('bqghd,bkgd->bqghk', qg, kw) + head_bias(rel_bias, t5_bucket(dist_w))[None]
    p_w = masked_softmax(s_w, mask_w[None, :, None, None, :])
    o_w = jnp.einsum('bqghk,bkgd->bqghd', p_w, vw)
    g = gates.reshape(bsz, tq, NSA_KV_GROUPS, NSA_HPG, 3)
    o = g[..., 0:1] * o_c + g[..., 1:2] * o_s + g[..., 2:3] * o_w
    return o.reshape(bsz, tq, NSA_HEADS * NSA_DH)


def nsa_prompt(q, gates, kv, rel_bias):
    f32 = jnp.float32
    bsz, s = q.shape[:2]
    kvf = kv.astype(f32)
    kc, vc, kb, vb = nsa_compress_and_block(kvf[:, :, :4])
    pad = ((0, 0), (NSA_WINDOW, 0), (0, 0), (0, 0))
    kw = jnp.pad(kvf[:, :, 4], pad)
    vw = jnp.pad(kvf[:, :, 5], pad)
    span = NSA_WINDOW + NSA_QBLOCK

    def body(i):
        s0 = i * NSA_QBLOCK
        qi = lax.dynamic_slice_in_dim(q, s0, NSA_QBLOCK, axis=1)
        gti = lax.dynamic_slice_in_dim(gates, s0, NSA_QBLOCK, axis=1)
        kwi = lax.dynamic_slice_in_dim(kw, s0, span, axis=1)
        vwi = lax.dynamic_slice_in_dim(vw, s0, span, axis=1)
        qpos = s0 + jnp.arange(NSA_QBLOCK)
        kwpos = s0 - NSA_WINDOW + jnp.arange(span)
        return nsa_block(qi, gti, qpos, kc, vc, kb, vb, kwi, vwi, kwpos, rel_bias)

    o = lax.map(body, jnp.arange(s // NSA_QBLOCK))
    return jnp.transpose(o, (1, 0, 2, 3)).reshape(bsz, s, NSA_HEADS * NSA_DH)


def nsa_sample(q, gates, kv_new, cache_kv, cache_win, page_table, rel_bias):
    f32 = jnp.float32
    db, t = q.shape[:2]
    past = page_table.shape[1] * cache_kv.shape[1]
    past_kv = cache_kv[page_table].reshape((db, past) + cache_kv.shape[2:])
    full = jnp.concatenate([past_kv.astype(f32), kv_new[:, :, :4].astype(f32)], axis=1)
    kc, vc, kb, vb = nsa_compress_and_block(full)
    wb = cache_win.shape[1]
    win = jnp.concatenate([cache_win, kv_new[:, :, 4:].astype(cache_win.dtype)], axis=1)
    winf = win.astype(f32)
    kwpos = past - wb + jnp.arange(wb + t)
    qpos = past + jnp.arange(t)
    o = nsa_block(q, gates, qpos, kc, vc, kb, vb, winf[:, :, 0], winf[:, :, 1], kwpos, rel_bias)
    return o, win[:, t:]


def rglru_mixer(xr, conv_buf, h0, conv_w, conv_b, wa, ba, wx, bx, lam):
    f32 = jnp.float32
    bsz, t, _ = xr.shape
    xc, new_buf = causal_conv(xr, conv_buf, conv_w, conv_b)
    xf = xc.astype(f32)
    xb = xf.reshape(bsz, t, RNN_BLOCKS, RNN_BW)
    r = jax.nn.sigmoid(jnp.einsum('btnd,nde->btne', xb, wa.astype(f32)).reshape(bsz, t, RNN_WIDTH) + ba.astype(f32))
    i = jax.nn.sigmoid(jnp.einsum('btnd,nde->btne', xb, wx.astype(f32)).reshape(bsz, t, RNN_WIDTH) + bx.astype(f32))
    log_a = -RG_C * r * jax.nn.softplus(-lam.astype(f32))
    a = jnp.exp(log_a)
    b = jnp.sqrt(jnp.maximum(-jnp.expm1(2.0 * log_a), 0.0)) * (i * xf)
    b = b.at[:, 0].add(a[:, 0] * h0.astype(f32))

    def combine(left, right):
        return left[0] * right[0], right[0] * left[1] + right[1]

    _, h = lax.associative_scan(combine, (a, b), axis=1)
    return h, new_buf, h[:, -1]


def moe_swiglu(h, router, w1, w3, w2):
    f32 = jnp.float32
    n, d = h.shape
    logits = (h @ router).astype(f32)
    top_val, top_idx = lax.top_k(logits, TOP_K)
    gate = jax.nn.softmax(top_val, axis=-1)
    n_assign = n * TOP_K
    e_flat = top_idx.reshape(n_assign)
    tok_flat = jnp.repeat(jnp.arange(n, dtype=jnp.int32), TOP_K)
    w_flat = gate.reshape(n_assign)
    order = jnp.argsort(e_flat, stable=True)
    e_sorted = e_flat[order]
    counts = jnp.zeros((N_EXPERTS,), jnp.int32).at[e_flat].add(1)
    padded = (counts + MOE_BLOCK - 1) // MOE_BLOCK * MOE_BLOCK
    start = jnp.cumsum(counts) - counts
    pend = jnp.cumsum(padded)
    pstart = pend - padded
    dest = pstart[e_sorted] + jnp.arange(n_assign) - start[e_sorted]
    n_blocks = -(-n_assign // MOE_BLOCK) + N_EXPERTS
    rows = n_blocks * MOE_BLOCK
    row_tok = jnp.zeros((rows,), jnp.int32).at[dest].set(tok_flat[order])
    row_w = jnp.zeros((rows,), f32).at[dest].set(w_flat[order])
    blk_e = jnp.minimum(jnp.searchsorted(pend, jnp.arange(n_blocks) * MOE_BLOCK, side='right'), N_EXPERTS - 1)
    xs = h[row_tok].reshape(n_blocks, MOE_BLOCK, d)

    def expert_block(args):
        xb, e = args
        return (jax.nn.silu(xb @ w1[e]) * (xb @ w3[e])) @ w2[e]

    yb = lax.map(expert_block, (xs, blk_e)).reshape(rows, d)
    y = jnp.zeros((n, d), f32).at[row_tok].add(yb.astype(f32) * row_w[:, None])
    return y.astype(h.dtype)


def trunk(x, c, nsa_mix, st, p):
    bsz, t, _ = x.shape
    f32 = jnp.float32
    new = {}
    for layer in range(DEPTH):
        mod = adaln_params(c, p['w_ada'][layer], p['b_ada'][layer])
        h = modulate(x, p['norm_mix'][layer], mod[0], mod[1])
        if layer % 2 == 0:
            qkv_raw, z, a_raw, b_raw, q_b, kv_b, g_b = _split_last(h @ p['w_in0'], P0_SIZES)
            o_a, new['gdn_conv'], new['gdn'] = gdn_mixer(qkv_raw, z, a_raw, b_raw, st['gdn_conv'], st['gdn'],
                                                         p['gdn_conv_w'], p['gdn_a_log'], p['gdn_dt_bias'], p['gdn_norm_w'])
            q_b = q_b.astype(f32).reshape(bsz, t, NSA_HEADS, NSA_DH) * NSA_DH ** -0.5
            g_b = jax.nn.sigmoid(g_b.astype(f32)).reshape(bsz, t, NSA_HEADS, 3)
            kv_b = kv_b.reshape(bsz, t, 6, NSA_KV_GROUPS, NSA_DH)
            o_b, new['nsa_win'] = nsa_mix(q_b, g_b, kv_b)
            new['nsa_kv'] = kv_b[:, :, :4]
            mix = jnp.concatenate([o_a, o_b], axis=-1).astype(x.dtype) @ p['w_out0']
            x = x + (mod[2] * mix).astype(x.dtype)
            h = modulate(x, p['norm_ffn'][layer], mod[3], mod[4])
            y = swiglu(h, p['ffn_w_gate'], p['ffn_w_up'], p['ffn_w_down'])
            x = x + (mod[5] * y).astype(x.dtype)
        else:
            gate_br, rec_br = _split_last(h @ p['w_in1'], (RNN_WIDTH, RNN_WIDTH))
            hr, new['lru_conv'], new['lru'] = rglru_mixer(rec_br, st['lru_conv'], st['lru'], p['lru_conv_w'], p['lru_conv_b'],
                                                          p['lru_wa'], p['lru_ba'], p['lru_wx'], p['lru_bx'], p['lru_lambda'])
            y = (jax.nn.gelu(gate_br.astype(f32)) * hr).astype(x.dtype) @ p['w_out1']
            x = x + (mod[2] * y).astype(x.dtype)
            h = modulate(x, p['norm_ffn'][layer], mod[3], mod[4])
            y = moe_swiglu(h.reshape(bsz * t, D_MODEL), p['moe_router'], p['moe_w1'], p['moe_w3'], p['moe_w2'])
            x = x + (mod[5] * y.reshape(bsz, t, D_MODEL)).astype(x.dtype)
    return rms_norm(x, p['norm_final']).astype(x.dtype), new


def setup_inputs(seed: int = 0) -> dict:
    key = jax.random.key(seed)
    ks = list(jax.random.split(key, 48))
    f32 = jnp.float32

    def nrm(shape, scale=1.0):
        return scale * jax.random.normal(ks.pop(), shape, f32)

    n_pages = PAST_LEN // PAGE_SIZE
    n_pool = (DEC_BATCH * n_pages * 5 + 3) // 4
    win_buf = min(NSA_WINDOW, PAST_LEN)
    page_table = jax.random.permutation(ks.pop(), n_pool)[:DEC_BATCH * n_pages].reshape(DEC_BATCH, n_pages).astype(jnp.int32)
    a_init = jax.random.uniform(ks.pop(), (GDN_HEADS,), f32, 1.0, 16.0)
    dt = jnp.exp(jax.random.uniform(ks.pop(), (GDN_HEADS,), f32, math.log(1e-3), math.log(1e-1)))
    a_target = jax.random.uniform(ks.pop(), (RNN_WIDTH,), f32, 0.9, 0.999)
    sig = a_target ** (1.0 / RG_C)
    d_in = D_MODEL ** -0.5
    return {
        'x_prompt': nrm((BATCH, SEQ, D_MODEL)),
        'x_sample': nrm((DEC_BATCH, DEC_SEQ, D_MODEL)),
        'c_prompt': nrm((BATCH, D_MODEL)),
        'c_sample': nrm((DEC_BATCH, D_MODEL)),
        'cache_nsa_kv': nrm((n_pool, PAGE_SIZE, 4, NSA_KV_GROUPS, NSA_DH)),
        'cache_nsa_win': nrm((DEC_BATCH, win_buf, 2, NSA_KV_GROUPS, NSA_DH)),
        'state_gdn': nrm((DEC_BATCH, GDN_HEADS, GDN_DK, GDN_DV), 0.3),
        'state_gdn_conv': nrm((DEC_BATCH, GDN_CONV - 1, GDN_CONV_CH)),
        'state_lru': nrm((DEC_BATCH, RNN_WIDTH), 0.5),
        'state_lru_conv': nrm((DEC_BATCH, RNN_CONV - 1, RNN_WIDTH)),
        'page_table': page_table,
        'rel_bias': nrm((REL_BUCKETS, NSA_HEADS), 0.3),
        'w_ada': nrm((DEPTH, D_MODEL, N_MOD * D_MODEL), 0.5 * d_in),
        'b_ada': nrm((DEPTH, N_MOD * D_MODEL), 0.02),
        'norm_mix': 1.0 + nrm((DEPTH, D_MODEL), 0.02),
        'norm_ffn': 1.0 + nrm((DEPTH, D_MODEL), 0.02),
        'norm_final': 1.0 + nrm((D_MODEL,), 0.02),
        'w_in0': nrm((D_MODEL, P0_WIDTH), d_in),
        'gdn_conv_w': nrm((GDN_CONV, GDN_CONV_CH), 0.5),
        'gdn_a_log': jnp.log(a_init),
        'gdn_dt_bias': dt + jnp.log(-jnp.expm1(-dt)),
        'gdn_norm_w': 1.0 + nrm((GDN_DV,), 0.02),
        'w_out0': nrm((MIX_WIDTH, D_MODEL), MIX_WIDTH ** -0.5),
        'ffn_w_gate': nrm((D_MODEL, FFN_DIM), d_in),
        'ffn_w_up': nrm((D_MODEL, FFN_DIM), d_in),
        'ffn_w_down': nrm((FFN_DIM, D_MODEL), FFN_DIM ** -0.5),
        'w_in1': nrm((D_MODEL, 2 * RNN_WIDTH), d_in),
        'lru_conv_w': nrm((RNN_CONV, RNN_WIDTH), 0.5),
        'lru_conv_b': nrm((RNN_WIDTH,), 0.02),
        'lru_wa': nrm((RNN_BLOCKS, RNN_BW, RNN_BW), RNN_BW ** -0.5),
        'lru_ba': nrm((RNN_WIDTH,), 0.02),
        'lru_wx': nrm((RNN_BLOCKS, RNN_BW, RNN_BW), RNN_BW ** -0.5),
        'lru_bx': nrm((RNN_WIDTH,), 0.02),
        'lru_lambda': jnp.log(sig) - jnp.log1p(-sig),
        'w_out1': nrm((RNN_WIDTH, D_MODEL), RNN_WIDTH ** -0.5),
        'moe_router': nrm((D_MODEL, N_EXPERTS), d_in),
        'moe_w1': nrm((N_EXPERTS, D_MODEL, EXPERT_DIM), d_in),
        'moe_w3': nrm((N_EXPERTS, D_MODEL, EXPERT_DIM), d_in),
        'moe_w2': nrm((N_EXPERTS, EXPERT_DIM, D_MODEL), EXPERT_DIM ** -0.5),
    }


def reference(x_prompt, x_sample, c_prompt, c_sample, cache_nsa_kv, cache_nsa_win, state_gdn, state_gdn_conv,
              state_lru, state_lru_conv, page_table, rel_bias, w_ada, b_ada, norm_mix, norm_ffn, norm_final,
              w_in0, gdn_conv_w, gdn_a_log, gdn_dt_bias, gdn_norm_w, w_out0, ffn_w_gate, ffn_w_up, ffn_w_down,
              w_in1, lru_conv_w, lru_conv_b, lru_wa, lru_ba, lru_wx, lru_bx, lru_lambda, w_out1,
              moe_router, moe_w1, moe_w3, moe_w2):
    p = {'w_ada': w_ada, 'b_ada': b_ada, 'norm_mix': norm_mix, 'norm_ffn': norm_ffn, 'norm_final': norm_final,
         'w_in0': w_in0, 'gdn_conv_w': gdn_conv_w, 'gdn_a_log': gdn_a_log, 'gdn_dt_bias': gdn_dt_bias,
         'gdn_norm_w': gdn_norm_w, 'w_out0': w_out0, 'ffn_w_gate': ffn_w_gate, 'ffn_w_up': ffn_w_up,
         'ffn_w_down': ffn_w_down, 'w_in1': w_in1, 'lru_conv_w': lru_conv_w, 'lru_conv_b': lru_conv_b,
         'lru_wa': lru_wa, 'lru_ba': lru_ba, 'lru_wx': lru_wx, 'lru_bx': lru_bx, 'lru_lambda': lru_lambda,
         'w_out1': w_out1, 'moe_router': moe_router, 'moe_w1': moe_w1, 'moe_w3': moe_w3, 'moe_w2': moe_w2}
    bsz = x_prompt.shape[0]
    st_p = {'gdn_conv': jnp.zeros((bsz, GDN_CONV - 1, GDN_CONV_CH), x_prompt.dtype),
            'gdn': jnp.zeros((bsz, GDN_HEADS, GDN_DK, GDN_DV), jnp.float32),
            'lru_conv': jnp.zeros((bsz, RNN_CONV - 1, RNN_WIDTH), x_prompt.dtype),
            'lru': jnp.zeros((bsz, RNN_WIDTH), jnp.float32)}
    st_s = {'gdn_conv': state_gdn_conv, 'gdn': state_gdn, 'lru_conv': state_lru_conv, 'lru': state_lru}

    def prompt_nsa(q, g, kv):
        keep = min(NSA_WINDOW, kv.shape[1])
        return nsa_prompt(q, g, kv, rel_bias), kv[:, kv.shape[1] - keep:, 4:]

    def sample_nsa(q, g, kv):
        return nsa_sample(q, g, kv, cache_nsa_kv, cache_nsa_win, page_table, rel_bias)

    y_prompt, pn = trunk(x_prompt, c_prompt, prompt_nsa, st_p, p)
    y_sample, sn = trunk(x_sample, c_sample, sample_nsa, st_s, p)
    return (y_prompt, y_sample,
            pn['nsa_kv'], pn['nsa_win'], pn['gdn'], pn['gdn_conv'], pn['lru'], pn['lru_conv'],
            sn['nsa_kv'], sn['nsa_win'], sn['gdn'], sn['gdn_conv'], sn['lru'], sn['lru_conv'])
```

```python
import math
import os
from contextlib import ExitStack

import numpy as np
import concourse.bass as bass
import concourse.mybir as mybir
from concourse.bass_utils import run_bass_kernel_spmd

F32 = mybir.dt.float32
BF16 = mybir.dt.bfloat16
I32 = mybir.dt.int32
AF = mybir.ActivationFunctionType
ALU = mybir.AluOpType
AX = mybir.AxisListType

ENGS = ["sync", "scalar", "vector", "gpsimd", "tensor"]
EPOCH = 30000
N_DSEM = 2

D = 1024
SEQ = 8192
NSAMP = 4
TT = SEQ + 64 * NSAMP
NTILE = TT // 128
EPS = 1e-6
P0W = 3360
NFM0 = 20
NTM0 = 1312
FFN = 2816
NPOOL = 2560


class Buf:
    __slots__ = ("name", "lw", "rd")

    def __init__(self, name):
        self.name = name
        self.lw = None
        self.rd = []


class Op:
    __slots__ = ("eng", "fn", "reads", "writes", "dma", "deps", "sig", "idx", "need_sig", "xdeps", "eidx", "filler")

    def __init__(self, eng, fn, reads, writes, dma):
        self.eng = eng
        self.fn = fn
        self.reads = reads
        self.writes = writes
        self.dma = dma
        self.deps = []
        self.xdeps = []
        self.sig = None
        self.need_sig = False
        self.eidx = 0
        self.filler = False


class Prog:
    def __init__(self, nc, stack):
        self.nc = nc
        self.stack = stack
        self.ops = []
        self.nbuf = 0
        self.ndma = 0
        self.last_eng = {}
        self.last_dsem = {}
        self.ecount = {e: 0 for e in ENGS}
        self.fill = {}

    def buf(self, name=None):
        self.nbuf += 1
        return Buf(name or f"b{self.nbuf}")

    def add(self, eng, fn, reads=(), writes=(), dma=False):
        op = Op(eng, fn, list(reads), list(writes), dma)
        op.idx = len(self.ops)
        self.ops.append(op)
        self.ecount[eng] += 1
        op.eidx = self.ecount[eng]
        if dma:
            s = self.ndma % N_DSEM
            self.ndma += 1
            op.sig = s
            self.last_dsem[s] = op
        else:
            self.last_eng[eng] = op
        return op

    def dma(self, eng, out, in_, reads=(), writes=(), **kw):
        return self.add(eng, lambda e: e.dma_start(out=out, in_=in_, **kw), reads, writes, dma=True)

    def barrier(self):
        prev = list(self.last_eng.values()) + list(self.last_dsem.values())
        for e in ENGS:
            op = self.add(e, lambda eng: eng.nop(), (), ())
            op.xdeps = [p for p in prev]

    def emit(self):
        nc = self.nc
        for op in self.ops:
            deps = {}
            for b in op.reads:
                if b.lw is not None:
                    deps[b.lw.idx] = (b.lw, True)
            for b in op.writes:
                if b.lw is not None and b.lw.idx not in deps:
                    deps[b.lw.idx] = (b.lw, b.lw.dma)
                for r in b.rd:
                    if r.idx not in deps:
                        deps[r.idx] = (r, False)
            for b in op.reads:
                b.rd.append(op)
            for b in op.writes:
                b.lw = op
                b.rd = []
            for d, raw in deps.values():
                if d is op:
                    continue
                if d.dma:
                    op.deps.append(d)
                elif d.eng != op.eng or op.dma:
                    op.deps.append(d)
                    d.need_sig = True
                elif raw and op.eng != "tensor":
                    if op.eidx - d.eidx <= 2:
                        op.deps.append(d)
                        d.need_sig = True
            for d in op.xdeps:
                if d is op:
                    continue
                op.deps.append(d)
                if not d.dma and d.eng != op.eng:
                    d.need_sig = True
        cnt = {e: 0 for e in ENGS}
        nsem_needed = {e: 0 for e in ENGS}
        dsem_cnt = [0] * N_DSEM
        dsem_last = [None] * N_DSEM
        for op in self.ops:
            if op.dma:
                s = op.sig
                prev = dsem_last[s]
                dsem_cnt[s] += 16
                op.sig = ("d", s, dsem_cnt[s])
                if prev is not None:
                    op.deps.append(prev)
                dsem_last[s] = op
            elif op.need_sig:
                c = cnt[op.eng]
                op.sig = (op.eng, c // EPOCH, c % EPOCH + 1)
                cnt[op.eng] = c + 1
                nsem_needed[op.eng] = c // EPOCH + 1
        sems = {}
        for e in ENGS:
            for i in range(nsem_needed[e]):
                sems[(e, i)] = self.stack.enter_context(nc.semaphore(f"s_{e}_{i}"))
        for s in range(N_DSEM):
            sems[("d", s)] = self.stack.enter_context(nc.semaphore(f"s_dma_{s}"))
        final_waits = [((op.sig[0], op.sig[1]), op.sig[2]) for op in dsem_last if op is not None]
        block = self.stack.enter_context(nc.Block())
        by_eng = {e: [op for op in self.ops if op.eng == e] for e in ENGS}

        def make(ename):
            def body(eng):
                waited = {}
                for op in by_eng[ename]:
                    for d in op.deps:
                        if d.sig is None:
                            continue
                        key = (d.sig[0], d.sig[1])
                        v = d.sig[2]
                        if waited.get(key, 0) >= v:
                            continue
                        waited[key] = v
                        eng.wait_ge(sems[key], v)
                    if op.filler and ename in self.fill:
                        self.fill[ename](eng)
                    ins = op.fn(eng)
                    if op.sig is not None:
                        ins.then_inc(sems[(op.sig[0], op.sig[1])], 16 if op.dma else 1)
                if ename == "sync":
                    for key, v in final_waits:
                        if waited.get(key, 0) >= v:
                            continue
                        eng.wait_ge(sems[key], v)
            return body

        block.sync(make("sync"))
        block.scalar(make("scalar"))
        block.vector(make("vector"))
        block.gpsimd(make("gpsimd"))
        block.tensor(make("tensor"))


class Arena:
    def __init__(self, nc, ncols):
        self.t = nc.alloc_sbuf_tensor("arena", [128, ncols], F32)
        self.n = ncols
        self.off = 0

    def mark(self):
        return self.off

    def release(self, m):
        self.off = m

    def alloc(self, cols, dtype=F32):
        n4 = cols if dtype in (F32, I32) else (cols + 1) // 2
        n4 = (n4 + 7) // 8 * 8
        assert self.off + n4 <= self.n, ("SBUF arena overflow", self.off, n4, self.n)
        ap = self.t[:, self.off:self.off + n4]
        self.off += n4
        if dtype != F32:
            ap = ap.bitcast(dtype)
        return ap[:, 0:cols]


def t5_bucket_np(dist):
    n = np.maximum(dist, 0)
    exact = 16
    nf = np.maximum(n, exact).astype(np.float32)
    large = exact + (np.log(nf / np.float32(exact)) / np.float32(math.log(2048 / exact)) * np.float32(16)).astype(np.int32)
    return np.where(n < exact, n, np.minimum(large, 31))


class Builder:
    def __init__(self, dbg=()):
        self.dbg = set(dbg)
        self.nc = bass.Bass("TRN2", target_bir_lowering=False)
        self.inputs = {}
        self.outputs = {}

    def din(self, name, shape, dtype=F32):
        t = self.nc.dram_tensor(name, list(shape), dtype, kind="ExternalInput")
        self.inputs[name] = (tuple(shape), dtype)
        return t

    def dout(self, name, shape, dtype=F32):
        t = self.nc.dram_tensor(name, list(shape), dtype, kind="ExternalOutput")
        self.outputs[name] = (tuple(shape), dtype)
        return t

    def dscr(self, name, shape, dtype=F32):
        if name in self.dbg:
            return self.dout(name, shape, dtype)
        return self.nc.dram_tensor(name, list(shape), dtype, kind="Internal")

    def build(self, phases=("p0", "a1")):
        nc = self.nc
        self.phases = tuple(phases)
        with ExitStack() as st:
            P = self.P = Prog(nc, st)
            A = self.A = Arena(nc, 51000)
            self.psum = []
            for i in range(8):
                t = nc.alloc_psum_tensor(f"psb{i}", [128, 512], F32)
                self.psum.append((t, P.buf(f"psum{i}")))
            self.psi = 0
            self.ps_n = 8
            self.declare_io()
            self.consts()
            if "p0" in phases:
                self.phase0()
                P.barrier()
            if "a1" in phases:
                self.phase_a1()
                P.barrier()
            if "gdn" in phases:
                self.phase_gdn()
                P.barrier()
            if "nsa" in phases:
                self.phase_nsa()
                P.barrier()
            if "b1" in phases:
                self.phase_b1a()
                P.barrier()
                self.phase_b1b()
                P.barrier()
            if "b2" in phases:
                self.phase_b2()
                P.barrier()
            if "c" in phases:
                self.phase_c1()
                P.barrier()
                self.phase_c2()
                P.barrier()
            P.emit()
        return nc

    def dump(self, name, ap, bufs, shape):
        if name not in self.dbg:
            return
        t = self.dout(name, shape)
        self.P.dma("sync", t.ap(), ap, reads=bufs)

    def ps(self):
        t, b = self.psum[self.psi % self.ps_n]
        self.psi += 1
        return t, b

    def declare_io(self):
        d = self.din
        self.xall = d("xall", [TT, D])
        self.c5T = d("c5T", [128, 8 * 5])
        self.w_ada = d("w_ada", [2, D, 6 * D])
        self.b_ada = d("b_ada", [2, 6 * D])
        self.norm_mix = d("norm_mix", [2, D])
        self.norm_ffn = d("norm_ffn", [2, D])
        self.norm_final = d("norm_final", [1, D])
        self.w_fm0 = d("w_fm0", [D, NFM0 * 128])
        self.w_tm0 = d("w_tm0", [D, NTM0])
        self.cw0 = d("cw0", [128, 12 * 4])
        self.gdn_ab = d("gdn_ab", [1, 8])
        self.sconv0 = d("sconv0", [NSAMP, 3, 1536])
        self.cwin = d("cwin", [NSAMP, 512, 256])
        self.rowvalid = d("rowvalid", [128, 2])
        self.ident = d("ident", [128, 128])
        self.ones = d("ones", [128, 128])
        self.sgdn = d("sgdn", [NSAMP, 4, 128, 128])
        self.gnw = d("gnw", [1, 128])
        self.umask = d("umask", [64, 192])
        self.o_pgdn = self.dout("o_pgdn", [4, 128, 128])
        self.idx_sel = d("idx_sel", [14, 128 * 128])
        self.idx_win = d("idx_win", [5, 128 * 128])
        self.idx_cmp = d("idx_cmp", [1, 128 * 256])
        self.relb33 = d("relb33", [33, 8])
        self.iota33 = d("iota33", [33, 1])
        self.wide = d("wide", [128, 128 * 65])
        self.vmfa = d("vmfa", [128, 512])
        self.TAB = self.dscr("TAB", [20, 8, 32768])
        self.w_out0 = d("w_out0", [D, D])
        self.ffn_wg = d("ffn_wg", [D, FFN])
        self.ffn_wu = d("ffn_wu", [D, FFN])
        self.ffn_wd = d("ffn_wd", [FFN, D])
        self.w_in1 = d("w_in1", [D, 2 * D])
        self.w_out1 = d("w_out1", [D, D])
        self.lru_wa = d("lru_wa", [8, 128, 128])
        self.lru_wx = d("lru_wx", [8, 128, 128])
        self.lru_vec = d("lru_vec", [128, 8 * 8])
        self.o_plru = self.dout("o_plru", [1, D])
        self.o_plconv = self.dout("o_plconv", [3, D])
        self.X1 = self.dscr("X1", [TT, D])
        self.H2T = self.dscr("H2T", [D, TT], BF16)
        self.X2 = self.dscr("X2", [TT, D])
        self.X3 = self.dscr("X3", [TT, D])
        self.moe_router = d("moe_router", [D, 8])
        self.cache2d = d("cache2d", [NPOOL * 128, 512])
        self.ptab = d("ptab", [NSAMP, 64], I32)
        self.iotap = d("iotap", [128, 1])
        self.hwide = d("hwide", [128, 256])
        self.slconv = d("slconv", [NSAMP, 3, D])
        self.slru = d("slru", [NSAMP, D])
        self.o_slru = self.dout("o_slru", [NSAMP, D])
        self.o_slconv = self.dout("o_slconv", [NSAMP, 3, D])
        if "c" in self.phases:
            self.moe_w1 = d("moe_w1", [8, D, 3584])
            self.moe_w3 = d("moe_w3", [8, D, 3584])
            self.moe_w2 = d("moe_w2", [8, 3584, D])
        self.o_y = self.dout("o_y", [TT, D])
        self.H3T = self.dscr("H3T", [D, TT], BF16)
        self.W8 = self.dscr("W8", [TT, 8])
        self.o_sgdn = self.dout("o_sgdn", [NSAMP, 4, 128, 128])
        o = self.dout
        self.o_pkv = o("o_pkv", [SEQ, 512])
        self.o_pwin = o("o_pwin", [512, 256])
        self.o_pgconv = o("o_pgconv", [3, 1536])
        self.o_skv = o("o_skv", [NSAMP, 512])
        self.o_swin = o("o_swin", [NSAMP, 512, 256])
        self.o_sgconv = o("o_sgconv", [NSAMP, 3, 1536])
        s = self.dscr
        self.modv = s("modv", [2, 5, 6 * D])
        self.QKV_T = s("QKV_T", [12, 128, TT])
        self.QN_T = s("QN_T", [4, 128, TT], BF16)
        self.KF_T = s("KF_T", [4, 128, TT])
        self.ZS = s("ZS", [TT, 512])
        self.KV_TOK = s("KV_TOK", [TT, 768])
        self.AB = s("AB", [TT, 8])
        self.GT = s("GT", [TT, 24])
        self.MIX_T = s("MIX_T", [D, TT], BF16)

    def consts(self):
        P, A = self.P, self.A
        self.identf = A.alloc(128); self.b_identf = P.buf("identf")
        self.identb = A.alloc(128, BF16); self.b_identb = P.buf("identb")
        self.onesf = A.alloc(128); self.b_onesf = P.buf("onesf")
        self.rowv = A.alloc(2); self.b_rowv = P.buf("rowv")
        P.dma("sync", self.identf, self.ident.ap(), writes=[self.b_identf])
        P.dma("gpsimd", self.identb, self.ident.ap(), writes=[self.b_identb])
        P.dma("sync", self.onesf, self.ones.ap(), writes=[self.b_onesf])
        P.dma("sync", self.rowv, self.rowvalid.ap(), writes=[self.b_rowv])
        fv = A.alloc(2); fs = A.alloc(2); fg = A.alloc(2)
        P.fill["vector"] = lambda e: e.memset(fv[:, 0:1], 0.0)
        P.fill["scalar"] = lambda e: e.memzero(fs[:, 0:1])
        P.fill["gpsimd"] = lambda e: e.memset(fg[:, 0:1], 0.0)
        self.epsc = A.alloc(1); self.b_epsc = P.buf("epsc")
        P.add("gpsimd", lambda e: e.memset(self.epsc, EPS), [], [self.b_epsc])

    def phase0(self):
        P, A = self.P, self.A
        m0 = A.mark()
        cT = A.alloc(40); b_cT = P.buf()
        P.dma("sync", cT, self.c5T.ap(), writes=[b_cT])
        csT = A.alloc(40); b_cs = P.buf()
        P.add("scalar", lambda e: e.activation(out=csT, in_=cT, func=AF.Silu), [b_cT], [b_cs])
        wt = [(A.alloc(8 * 512), P.buf()) for _ in range(3)]
        bt = [(A.alloc(512), P.buf()) for _ in range(2)]
        mo = [(A.alloc(512), P.buf()) for _ in range(2)]
        it = 0
        for l in range(2):
            for j in range(12):
                w, bw = wt[it % 3]
                bb, bbb = bt[it % 2]
                m, bm = mo[it % 2]
                src = self.w_ada[l, :, j * 512:(j + 1) * 512].rearrange("(k p) n -> p k n", p=128)
                P.dma("sync" if it % 2 == 0 else "scalar", w.rearrange("p (k n) -> p k n", k=8), src, writes=[bw])
                P.dma("sync", bb[0:5, :], bass.AP(self.b_ada, l * 6 * D + j * 512, [[0, 5], [1, 512]]), writes=[bbb])
                pt, bp = self.ps()

                def mm(e, w=w, pt=pt):
                    ins = None
                    for k in range(8):
                        ins = e.matmul(pt[0:5, :], lhsT=csT[:, k * 5:(k + 1) * 5], rhs=w[:, k * 512:(k + 1) * 512],
                                       start=(k == 0), stop=(k == 7))
                    return ins
                P.add("tensor", mm, [b_cs, bw], [bp])
                P.add("vector", lambda e, m=m, pt=pt, bb=bb: e.tensor_tensor(out=m[0:5, :], in0=pt[0:5, :], in1=bb[0:5, :], op=ALU.add),
                      [bp, bbb], [bm])
                P.dma("sync", self.modv[l, :, j * 512:(j + 1) * 512], m[0:5, :], reads=[bm], writes=[])
                it += 1
        A.release(m0)

    def mod_tiles(self, layer, which, need_gate=False, only_gate=False):
        P, A = self.P, self.A
        gain = (self.norm_mix if which == 0 else self.norm_ffn)
        res = {}
        gt = A.alloc(D); b_gt = P.buf()
        P.dma("sync", gt, bass.AP(gain, layer * D, [[0, 128], [1, D]]), writes=[b_gt])
        for ty in range(3):
            halves = [(0, 128, 0)] if ty == 0 else [(0, 64, 1 + 2 * (ty - 1)), (64, 128, 2 + 2 * (ty - 1))]
            if only_gate:
                Gt = A.alloc(D); bGt = P.buf()
                for (p0, p1, r) in halves:
                    base = (layer * 5 + r) * 6 * D + which * 3 * D
                    P.dma("sync", Gt[p0:p1, :], bass.AP(self.modv, base + 2 * D, [[0, p1 - p0], [1, D]]), writes=[bGt])
                res[ty] = [None, None, None, None, Gt, bGt]
                continue
            G = A.alloc(D); Sh = A.alloc(D); bG = P.buf(); bS = P.buf()
            for (p0, p1, r) in halves:
                base = (layer * 5 + r) * 6 * D + which * 3 * D
                P.dma("sync", Sh[p0:p1, :], bass.AP(self.modv, base, [[0, p1 - p0], [1, D]]), writes=[bS])
                P.dma("sync", G[p0:p1, :], bass.AP(self.modv, base + D, [[0, p1 - p0], [1, D]]), writes=[bG])
            P.add("vector", lambda e, G=G: e.scalar_tensor_tensor(out=G, in0=G, scalar=1.0, in1=gt, op0=ALU.add, op1=ALU.mult),
                  [bG, b_gt], [bG])
            ent = [G, Sh, bG, bS]
            if need_gate:
                Gt = A.alloc(D); bGt = P.buf()
                for (p0, p1, r) in halves:
                    base = (layer * 5 + r) * 6 * D + which * 3 * D
                    P.dma("sync", Gt[p0:p1, :], bass.AP(self.modv, base + 2 * D, [[0, p1 - p0], [1, D]]), writes=[bGt])
                ent += [Gt, bGt]
            res[ty] = ent
        return res

    def tile_type(self, t):
        return 0 if t < 64 else t - 63

    def norm_mod_transpose(self, xt, b_x, mt, hT, b_hT, col0, scr):
        P = self.P
        G, Sh, bG, bS = mt[0], mt[1], mt[2], mt[3]
        junk, b_junk, ss, b_ss, rstd, b_rstd, h32, b_h32, hb, b_hb = scr
        P.add("gpsimd", lambda e: e.memset(ss, 0.0), [], [b_ss])
        P.add("scalar", lambda e: e.activation(out=junk, in_=xt, func=AF.Square, accum_out=ss), [b_x, b_ss], [b_junk, b_ss])
        P.add("scalar", lambda e: e.activation(out=rstd, in_=ss, func=AF.Sqrt, bias=self.epsc, scale=1.0 / D), [b_ss, self.b_epsc], [b_rstd])
        P.add("vector", lambda e: e.reciprocal(out=rstd, in_=rstd), [b_rstd], [b_rstd])
        P.add("vector", lambda e: e.scalar_tensor_tensor(out=h32, in0=xt, scalar=rstd, in1=G, op0=ALU.mult, op1=ALU.mult),
              [b_x, b_rstd, bG], [b_h32])
        P.add("gpsimd", lambda e: e.tensor_tensor(out=hb, in0=h32, in1=Sh, op=ALU.add), [b_h32, bS], [b_hb])
        pt, bp = self.ps()
        ptb = pt[:, :].bitcast(BF16)

        def tr(e):
            ins = None
            for k in range(8):
                ins = e.transpose(out=ptb[:, k * 128:(k + 1) * 128], in_=hb[:, k * 128:(k + 1) * 128], identity=self.identb)
            return ins
        P.add("tensor", tr, [b_hb, self.b_identb], [bp])
        P.add("scalar", lambda e: e.copy(out=hT[:, :, col0:col0 + 128], in_=ptb.rearrange("p (k n) -> p k n", k=8)),
              [bp], [b_hT])

    def phase_a1(self):
        P, A = self.P, self.A
        nc = self.nc
        m0 = A.mark()
        wfm = A.alloc(8 * NFM0 * 128, BF16); b_wfm = P.buf()
        wtm = A.alloc(8 * NTM0, BF16); b_wtm = P.buf()
        wfm3 = wfm.rearrange("p (k n) -> p k n", k=8)
        wtm3 = wtm.rearrange("p (k n) -> p k n", k=8)
        for k in range(8):
            P.dma("gpsimd", wfm3[:, k, :], self.w_fm0[k * 128:(k + 1) * 128, :], writes=[b_wfm])
            P.dma("gpsimd", wtm3[:, k, :], self.w_tm0[k * 128:(k + 1) * 128, :], writes=[b_wtm])
        cw = A.alloc(48); b_cw = P.buf()
        P.dma("sync", cw, self.cw0.ap(), writes=[b_cw])
        ab = A.alloc(8); b_ab = P.buf()
        P.dma("sync", ab, bass.AP(self.gdn_ab, 0, [[0, 128], [1, 8]]), writes=[b_ab])
        nexpA = A.alloc(4); b_nexpA = P.buf()
        P.add("scalar", lambda e: e.activation(out=nexpA, in_=ab[:, 0:4], func=AF.Exp), [b_ab], [b_nexpA])
        P.add("vector", lambda e: e.tensor_scalar(out=nexpA, in0=nexpA, scalar1=-1.0, scalar2=None, op0=ALU.mult), [b_nexpA], [b_nexpA])
        mt = self.mod_tiles(0, 0)
        xts = [(A.alloc(D), P.buf()) for _ in range(2)]
        scrs = []
        for _ in range(2):
            scrs.append((A.alloc(D, BF16), P.buf(), A.alloc(1), P.buf(), A.alloc(1), P.buf(),
                         A.alloc(D), P.buf(), A.alloc(D, BF16), P.buf()))
        hTs = [(A.alloc(8 * 512, BF16).rearrange("p (k n) -> p k n", k=8), P.buf()) for _ in range(2)]
        xcs = [(A.alloc(12 * 515).rearrange("p (t n) -> p t n", t=12), [P.buf() for _ in range(12)])]
        ycs = [(A.alloc(512), P.buf()) for _ in range(3)]
        sqs = [(A.alloc(512), P.buf()) for _ in range(2)]
        rns = [(A.alloc(512), P.buf()) for _ in range(2)]
        ynf = [(A.alloc(512), P.buf()) for _ in range(3)]
        ynb = [(A.alloc(512, BF16), P.buf()) for _ in range(2)]
        ztm = [(A.alloc(512), P.buf()) for _ in range(2)]
        kvt = [(A.alloc(768), P.buf()) for _ in range(2)]
        abt = [(A.alloc(32), P.buf()) for _ in range(2)]
        abo = [(A.alloc(32), P.buf()) for _ in range(2)]
        xc0, bxc0 = xcs[0]
        P.add("gpsimd", lambda e: e.memset(xc0[:, :, 0:3], 0.0), [], bxc0)
        b_dram = {n: P.buf("dram_" + n) for n in ["QKV_T", "QN_T", "KF_T", "ZS", "KV_TOK", "AB", "GT"]}
        self.b_dram = b_dram
        import os
        nst = int(os.environ.get('A1_NST', '17'))
        ti = 0
        yi = 0
        for s in range(nst):
            W = 512 if s < 16 else 256
            c0 = s * 512
            ntl = W // 128
            hT, b_hT = hTs[s % 2]
            xc, bxc = xcs[0]
            xcn, bxcn = xcs[0]
            for tl in range(ntl):
                t = s * 4 + tl
                xt, b_x = xts[ti % 2]
                scr = scrs[ti % 2]
                ti += 1
                P.dma("sync", xt, self.xall[t * 128:(t + 1) * 128, :], writes=[b_x])
                self.norm_mod_transpose(xt, b_x, mt[self.tile_type(t)], hT, b_hT, tl * 128, scr)
            if s == 16 and not os.environ.get('NO_EW'):
                for sm in range(NSAMP):
                    for ct in range(12):
                        src = bass.AP(self.sconv0, sm * 3 * 1536 + ct * 128, [[1, 128], [1536, 3]])
                        P.dma("sync", xc[:, ct, 64 * sm:64 * sm + 3], src, writes=[bxc[ct]], allow_slow_non_contiguous=True)
            for ct in range(NFM0):
                pt, bp = self.ps()

                def mm(e, pt=pt, ct=ct, hT=hT, W=W):
                    ins = None
                    for k in range(8):
                        ins = e.matmul(pt[:, 0:W], lhsT=wfm3[:, k, ct * 128:(ct + 1) * 128], rhs=hT[:, k, 0:W],
                                       start=(k == 0), stop=(k == 7))
                    return ins
                P.add("tensor", mm, [b_wfm, b_hT], [bp])
                if ct < 12:
                    if s == 16:
                        for sm in range(NSAMP):
                            lo = 64 * sm + 3
                            hi = 64 * sm + 64 if sm < NSAMP - 1 else 64 * sm + 64 + 3
                            hi = min(hi, W + 3)
                            P.add("scalar", lambda e, pt=pt, ct=ct, lo=lo, hi=hi: e.copy(out=xc[:, ct, lo:hi], in_=pt[:, lo - 3:hi - 3]),
                                  [bp], [bxc[ct]])
                    else:
                        P.add("scalar", lambda e, pt=pt, ct=ct, W=W: e.copy(out=xc[:, ct, 3:3 + W], in_=pt[:, 0:W]), [bp], [bxc[ct]])
                    yc, b_yc = ycs[yi % 3]
                    yn, b_yn = ynf[yi % 3]
                    yi += 1
                    wcol = lambda j, ct=ct: cw[:, ct * 4 + j:ct * 4 + j + 1]
                    P.add("vector", lambda e, yc=yc, ct=ct, W=W, wcol=wcol: e.tensor_scalar(
                        out=yc[:, 0:W], in0=xc[:, ct, 3:3 + W], scalar1=wcol(3), scalar2=None, op0=ALU.mult),
                        [bxc[ct], b_cw], [b_yc])
                    for j in range(3):
                        P.add("vector", lambda e, yc=yc, ct=ct, W=W, j=j, wcol=wcol: e.scalar_tensor_tensor(
                            out=yc[:, 0:W], in0=xc[:, ct, j:j + W], scalar=wcol(j), in1=yc[:, 0:W], op0=ALU.mult, op1=ALU.add),
                            [bxc[ct], b_yc, b_cw], [b_yc])
                    if s < 15:
                        P.add("gpsimd", lambda e, ct=ct, W=W: e.tensor_copy(out=xcn[:, ct, 0:3], in_=xc[:, ct, W:W + 3]),
                              [bxc[ct]], [bxcn[ct]])
                    if s == 15 and not os.environ.get('NO_EW'):
                        dst = bass.AP(self.o_pgconv, ct * 128, [[1, 128], [1536, 3]])
                        P.dma("sync", dst, xc[:, ct, W:W + 3], reads=[bxc[ct]], allow_slow_non_contiguous=True)
                    if s == 16 and not os.environ.get('NO_EW'):
                        for sm in range(NSAMP):
                            dst = bass.AP(self.o_sgconv, sm * 3 * 1536 + ct * 128, [[1, 128], [1536, 3]])
                            P.dma("sync", dst, xc[:, ct, 64 * sm + 1:64 * sm + 4], reads=[bxc[ct]], allow_slow_non_contiguous=True)
                    P.add("scalar", lambda e, yc=yc, W=W: e.activation(out=yc[:, 0:W], in_=yc[:, 0:W], func=AF.Silu), [b_yc], [b_yc])
                    if ct < 8:
                        sq, b_sq = sqs[yi % 2]
                        rn, b_rn = rns[yi % 2]
                        P.add("gpsimd", lambda e, sq=sq, yc=yc, W=W: e.tensor_tensor(out=sq[:, 0:W], in0=yc[:, 0:W], in1=yc[:, 0:W], op=ALU.mult),
                              [b_yc], [b_sq])
                        p2, bp2 = self.ps()
                        P.add("tensor", lambda e, p2=p2, sq=sq, W=W: e.matmul(p2[:, 0:W], lhsT=self.onesf, rhs=sq[:, 0:W], start=True, stop=True),
                              [b_sq, self.b_onesf], [bp2])
                        P.add("scalar", lambda e, rn=rn, p2=p2, W=W: e.activation(out=rn[:, 0:W], in_=p2[:, 0:W], func=AF.Sqrt, bias=self.epsc, scale=1.0),
                              [bp2, self.b_epsc], [b_rn])
                        P.add("vector", lambda e, rn=rn, W=W: e.reciprocal(out=rn[:, 0:W], in_=rn[:, 0:W]), [b_rn], [b_rn])
                        sc = (128.0 ** -0.5) if ct < 4 else 1.0
                        P.add("vector", lambda e, yn=yn, yc=yc, rn=rn, W=W, sc=sc: e.scalar_tensor_tensor(
                            out=yn[:, 0:W], in0=yc[:, 0:W], scalar=sc, in1=rn[:, 0:W], op0=ALU.mult, op1=ALU.mult),
                            [b_yc, b_rn], [b_yn])
                        P.dma("sync", self.QKV_T[ct, :, c0:c0 + W], yn[:, 0:W], reads=[b_yn])
                    else:
                        P.dma("sync", self.QKV_T[ct, :, c0:c0 + W], yc[:, 0:W], reads=[b_yc])
                elif ct < 16:
                    yb, b_yb = ynb[ct % 2]
                    P.add("scalar", lambda e, yb=yb, pt=pt, W=W: e.mul(out=yb[:, 0:W], in_=pt[:, 0:W], mul=0.125),
                          [bp], [b_yb])
                    P.dma("sync", self.QN_T[ct - 12, :, c0:c0 + W], yb[:, 0:W], reads=[b_yb])
                else:
                    yn, b_yn = ynf[yi % 3]
                    yi += 1
                    P.add("scalar", lambda e, yn=yn, pt=pt, W=W: e.copy(out=yn[:, 0:W], in_=pt[:, 0:W]), [bp], [b_yn])
                    P.dma("sync", self.KF_T[ct - 16, :, c0:c0 + W], yn[:, 0:W], reads=[b_yn])
            for tl in range(ntl):
                t = s * 4 + tl
                r0 = t * 128
                z, b_z = ztm[t % 2]
                kv, b_kv = kvt[t % 2]
                a_in, b_ain = abt[t % 2]
                a_out, b_aout = abo[t % 2]
                chunks = [(0, 512), (512, 1024), (1024, 1312)]
                for ci, (n0, n1) in enumerate(chunks):
                    pt, bp = self.ps()

                    def mm(e, pt=pt, n0=n0, n1=n1, hT=hT, tl=tl):
                        ins = None
                        for k in range(8):
                            ins = e.matmul(pt[:, 0:n1 - n0], lhsT=hT[:, k, tl * 128:(tl + 1) * 128], rhs=wtm3[:, k, n0:n1],
                                           start=(k == 0), stop=(k == 7))
                        return ins
                    P.add("tensor", mm, [b_wtm, b_hT], [bp])
                    if ci == 0:
                        P.add("scalar", lambda e, z=z, pt=pt: e.activation(out=z, in_=pt[:, 0:512], func=AF.Silu), [bp], [b_z])
                        P.dma("sync", self.ZS[r0:r0 + 128, :], z, reads=[b_z])
                    elif ci == 1:
                        P.add("vector", lambda e, kv=kv, pt=pt: e.tensor_copy(out=kv[:, 0:512], in_=pt[:, 0:512]), [bp], [b_kv])
                    else:
                        P.add("vector", lambda e, kv=kv, pt=pt: e.tensor_copy(out=kv[:, 512:768], in_=pt[:, 0:256]), [bp], [b_kv])
                        P.add("scalar", lambda e, a_in=a_in, pt=pt: e.copy(out=a_in, in_=pt[:, 256:288]), [bp], [b_ain])
                P.dma("sync", self.KV_TOK[r0:r0 + 128, :], kv, reads=[b_kv])
                if t < 64:
                    P.dma("sync", self.o_pkv[r0:r0 + 128, :], kv[:, 0:512], reads=[b_kv])
                    if t >= 60:
                        P.dma("sync", self.o_pwin[(t - 60) * 128:(t - 59) * 128, :], kv[:, 512:768], reads=[b_kv])
                else:
                    for hlf in range(2):
                        sm = (t - 64) * 2 + hlf
                        P.dma("sync", self.o_skv[sm:sm + 1, :], kv[64 * hlf:64 * hlf + 1, 0:512], reads=[b_kv])
                        P.dma("sync", self.o_swin[sm, 511:512, :], kv[64 * hlf:64 * hlf + 1, 512:768], reads=[b_kv])
                rv = self.rowv[:, 0:1] if t < 64 else self.rowv[:, 1:2]
                P.add("vector", lambda e, a_in=a_in, a_out=a_out: e.tensor_tensor(out=a_out[:, 0:4], in0=a_in[:, 0:4], in1=ab[:, 4:8], op=ALU.add),
                      [b_ain, b_ab], [b_aout])
                P.add("scalar", lambda e, a_out=a_out: e.activation(out=a_out[:, 0:4], in_=a_out[:, 0:4], func=AF.Exp), [b_aout], [b_aout])
                P.add("scalar", lambda e, a_out=a_out: e.activation(out=a_out[:, 0:4], in_=a_out[:, 0:4], func=AF.Ln, bias=1.0), [b_aout], [b_aout])
                P.add("vector", lambda e, a_out=a_out, rv=rv: e.scalar_tensor_tensor(out=a_out[:, 0:4], in0=a_out[:, 0:4], scalar=rv, in1=nexpA,
                                                                                  op0=ALU.mult, op1=ALU.mult),
                      [b_aout, b_nexpA, self.b_rowv], [b_aout])
                P.add("scalar", lambda e, a_in=a_in, a_out=a_out: e.activation(out=a_out[:, 4:32], in_=a_in[:, 4:32], func=AF.Sigmoid),
                      [b_ain], [b_aout])
                P.add("vector", lambda e, a_out=a_out, rv=rv: e.tensor_scalar(out=a_out[:, 4:8], in0=a_out[:, 4:8], scalar1=rv, scalar2=None, op0=ALU.mult),
                      [b_aout, self.b_rowv], [b_aout])
                P.dma("sync", self.AB[r0:r0 + 128, :], a_out[:, 0:8], reads=[b_aout])
                P.dma("sync", self.GT[r0:r0 + 128, :], a_out[:, 8:32], reads=[b_aout])
        for sm in range(NSAMP):
            P.dma("sync", self.o_swin[sm, 0:511, :], self.cwin[sm, 1:512, :])
        A.release(m0)

    def phase_gdn(self):
        P, A = self.P, self.A
        m0 = A.mark()
        V = lambda fn, r, w: P.add("vector", fn, r, w)
        S_ = lambda fn, r, w: P.add("scalar", fn, r, w)
        G_ = lambda fn, r, w: P.add("gpsimd", fn, r, w)
        T_ = lambda fn, r, w: P.add("tensor", fn, r, w)
        idf, b_idf = self.identf, self.b_identf
        ones, b_ones = self.onesf, self.b_onesf
        um = A.alloc(192); b_um = P.buf()
        P.dma("sync", um[0:64, :], self.umask.ap(), writes=[b_um])
        m_up = um[0:64, 0:64]; m_slo = um[0:64, 64:128]; m_lo = um[0:64, 128:192]
        gnw = A.alloc(128); b_gnw = P.buf()
        P.dma("sync", gnw[0:64, :], bass.AP(self.gnw, 0, [[0, 64], [1, 128]]), writes=[b_gnw])
        St = A.alloc(512); b_S = P.buf()
        S3 = St.rearrange("p (h e) -> p h e", h=4)
        def rot(n, cols, dt=F32):
            return [(A.alloc(cols, dt), P.buf()) for _ in range(n)]
        qkv_b = rot(2, 12 * 512)
        ab_b = rot(2, 8 * 8)
        zs_b = rot(2, 8 * 512)
        mix_b = rot(2, 4 * 512, BF16)
        ktok_b = rot(2, 512); vtok_b = rot(2, 512)
        lau_b = rot(2, 256)
        gsm_b = rot(2, 16)
        gc_b = rot(2, 4)
        x_b = rot(2, 256)
        gamT_b = rot(2, 256); gam_b = rot(2, 256); egbc_b = rot(2, 256)
        A_b = rot(3, 512)
        z_b = rot(3, 1024)
        wT_b = rot(2, 256)
        aqk_b = rot(2, 256)
        qg_b = rot(2, 256); kd_b = rot(2, 512)
        u_b = rot(2, 512)
        o_b = rot(2, 512); ob_b = rot(2, 512, BF16)
        st_b = rot(2, 8)
        cnt = [0]

        def nxt(lst):
            cnt[0] += 1
            return lst[cnt[0] % len(lst)]

        seqs = [("prompt", 0, 128, None)] + [("s%d" % sm, SEQ + 64 * sm, 1, sm) for sm in range(NSAMP)]
        for (nm, col0, nch, sm) in seqs:
            if sm is None:
                G_(lambda e: e.memset(St, 0.0), [], [b_S])
            else:
                P.dma("sync", S3, self.sgdn[sm].rearrange("h d e -> d h e"), writes=[b_S])
            nsc = (nch + 7) // 8
            for sc in range(nsc):
                c0 = col0 + sc * 512
                ncs = min(8, nch - sc * 8)
                Wc = ncs * 64
                qkv, b_qkv = qkv_b[sc % 2]
                q3 = qkv.rearrange("p (t n) -> p t n", t=12)
                abt, b_abt = ab_b[sc % 2]
                ab3 = abt.rearrange("p (c n) -> p c n", c=8)
                zs, b_zs = zs_b[sc % 2]
                zs3 = zs.rearrange("p (c n) -> p c n", c=8)
                mix, b_mix = mix_b[sc % 2]
                mix3 = mix.rearrange("p (h n) -> p h n", h=4)
                for t3 in range(3):
                    P.dma("sync", q3[:, 4 * t3:4 * t3 + 4, 0:Wc], self.QKV_T[4 * t3:4 * t3 + 4, :, c0:c0 + Wc].rearrange("t p n -> p t n"), writes=[b_qkv])
                P.dma("sync", ab3[0:64, 0:ncs, :], self.AB[c0:c0 + Wc, :].rearrange("(c p) n -> p c n", p=64), writes=[b_abt])
                P.dma("sync", zs3[0:64, 0:ncs, :], self.ZS[c0:c0 + Wc, :].rearrange("(c p) n -> p c n", p=64), writes=[b_zs])
                for ci in range(ncs):
                    cs = slice(ci * 64, ci * 64 + 64)
                    la = ab3[0:64, ci, 0:4]
                    bt = ab3[0:64, ci, 4:8]
                    ktok, b_ktok = nxt(ktok_b); vtok, b_vtok = nxt(vtok_b)
                    pk, bpk = self.ps(); pv, bpv = self.ps()

                    def trkv(e, pk=pk, pv=pv, cs=cs, q3=q3):
                        ins = None
                        for h in range(4):
                            e.transpose(out=pk[0:64, h * 128:(h + 1) * 128], in_=q3[:, 4 + h, cs], identity=idf)
                            ins = e.transpose(out=pv[0:64, h * 128:(h + 1) * 128], in_=q3[:, 8 + h, cs], identity=idf)
                        return ins
                    T_(trkv, [b_qkv, b_idf], [bpk, bpv])
                    S_(lambda e, ktok=ktok, pk=pk: e.copy(out=ktok[0:64, :], in_=pk[0:64, :]), [bpk], [b_ktok])
                    S_(lambda e, vtok=vtok, pv=pv: e.copy(out=vtok[0:64, :], in_=pv[0:64, :]), [bpv], [b_vtok])
                    lau, b_lau = nxt(lau_b)
                    for h in range(4):
                        V(lambda e, lau=lau, h=h, la=la: e.tensor_scalar(out=lau[0:64, h * 64:(h + 1) * 64], in0=m_up, scalar1=la[:, h:h + 1], scalar2=None, op0=ALU.mult),
                          [b_um, b_abt], [b_lau])
                    pg, bpg = self.ps()

                    def gmm(e, pg=pg, lau=lau, la=la):
                        e.matmul(pg[:, 0:256], lhsT=ones[0:64, :], rhs=lau[0:64, :], start=True, stop=True)
                        e.matmul(pg[0:64, 256:260], lhsT=m_up, rhs=la, start=True, stop=True)
                        return e.matmul(pg[:, 264:268], lhsT=ones[0:64, :], rhs=la, start=True, stop=True)
                    T_(gmm, [b_lau, b_ones, b_um, b_abt], [bpg])
                    gsm, b_gsm = nxt(gsm_b); gc, b_gc = nxt(gc_b)
                    V(lambda e, gsm=gsm, pg=pg: e.tensor_copy(out=gsm[0:64, 0:4], in_=pg[0:64, 256:260]), [bpg], [b_gsm])
                    S_(lambda e, gsm=gsm, pg=pg: e.activation(out=gsm[0:64, 4:8], in_=pg[0:64, 256:260], func=AF.Exp), [bpg], [b_gsm])
                    V(lambda e, gsm=gsm, pg=pg: e.tensor_tensor(out=gsm[0:64, 8:12], in0=pg[0:64, 264:268], in1=gsm[0:64, 0:4], op=ALU.subtract), [bpg, b_gsm], [b_gsm])
                    S_(lambda e, gsm=gsm: e.activation(out=gsm[0:64, 8:12], in_=gsm[0:64, 8:12], func=AF.Exp), [b_gsm], [b_gsm])
                    S_(lambda e, gc=gc, pg=pg: e.activation(out=gc, in_=pg[:, 264:268], func=AF.Exp), [bpg], [b_gc])
                    egbc, b_egbc = nxt(egbc_b)
                    S_(lambda e, egbc=egbc, pg=pg: e.activation(out=egbc, in_=pg[:, 0:256], func=AF.Exp), [bpg], [b_egbc])
                    X, b_X = nxt(x_b)
                    for h in range(4):
                        V(lambda e, X=X, pg=pg, gsm=gsm, h=h: e.tensor_scalar(out=X[0:64, h * 64:(h + 1) * 64], in0=pg[0:64, h * 64:(h + 1) * 64],
                                                                             scalar1=gsm[0:64, h:h + 1], scalar2=None, op0=ALU.subtract),
                          [bpg, b_gsm], [b_X])
                    gamT, b_gamT = nxt(gamT_b); gam, b_gam = nxt(gam_b)
                    V(lambda e, gamT=gamT, X=X: e.tensor_scalar(out=gamT[0:64, :], in0=X[0:64, :], scalar1=0.0, scalar2=None, op0=ALU.min), [b_X], [b_gamT])
                    V(lambda e, gam=gam, X=X: e.tensor_scalar(out=gam[0:64, :], in0=X[0:64, :], scalar1=-1.0, scalar2=0.0, op0=ALU.mult, op1=ALU.min), [b_X], [b_gam])
                    S_(lambda e, gamT=gamT: e.activation(out=gamT[0:64, :], in_=gamT[0:64, :], func=AF.Exp), [b_gamT], [b_gamT])
                    S_(lambda e, gam=gam: e.activation(out=gam[0:64, :], in_=gam[0:64, :], func=AF.Exp), [b_gam], [b_gam])
                    for h in range(4):
                        G_(lambda e, gamT=gamT, h=h: e.tensor_tensor(out=gamT[0:64, h * 64:(h + 1) * 64], in0=gamT[0:64, h * 64:(h + 1) * 64], in1=m_up, op=ALU.mult),
                           [b_gamT, b_um], [b_gamT])
                    pkk, bpkk = self.ps()

                    def kkmm(e, pkk=pkk, cs=cs, q3=q3):
                        ins = None
                        for h in range(4):
                            e.matmul(pkk[0:64, h * 64:(h + 1) * 64], lhsT=q3[:, 4 + h, cs], rhs=q3[:, 4 + h, cs], start=True, stop=True)
                            ins = e.matmul(pkk[0:64, 256 + h * 64:256 + (h + 1) * 64], lhsT=q3[:, 4 + h, cs], rhs=q3[:, h, cs], start=True, stop=True)
                        return ins
                    T_(kkmm, [b_qkv], [bpkk])
                    Al, b_Al = nxt(A_b)
                    for h in range(4):
                        V(lambda e, Al=Al, gam=gam, h=h: e.tensor_tensor(out=gam[0:64, h * 64:(h + 1) * 64], in0=gam[0:64, h * 64:(h + 1) * 64], in1=m_slo, op=ALU.mult),
                          [b_gam, b_um], [b_gam])
                        V(lambda e, Al=Al, gam=gam, pkk=pkk, h=h, bt=bt: e.scalar_tensor_tensor(out=Al[0:64, h * 64:(h + 1) * 64], in0=pkk[0:64, h * 64:(h + 1) * 64],
                                                                                              scalar=bt[:, h:h + 1], in1=gam[0:64, h * 64:(h + 1) * 64],
                                                                                              op0=ALU.mult, op1=ALU.mult),
                          [bpkk, b_gam, b_abt], [b_Al])
                    aqk, b_aqk = nxt(aqk_b)
                    V(lambda e, aqk=aqk, pkk=pkk, gamT=gamT: e.tensor_tensor(out=aqk[0:64, :], in0=pkk[0:64, 256:512], in1=gamT[0:64, :], op=ALU.mult),
                      [bpkk, b_gamT], [b_aqk])
                    pat, bpat = self.ps()

                    def trA(e, pat=pat, Al=Al):
                        ins = None
                        for h in range(4):
                            ins = e.transpose(out=pat[0:64, h * 64:(h + 1) * 64], in_=Al[0:64, h * 64:(h + 1) * 64], identity=idf[0:64, 0:64])
                        return ins
                    T_(trA, [b_Al, b_idf], [bpat])
                    S_(lambda e, Al=Al, pat=pat: e.copy(out=Al[0:64, 256:512], in_=pat[0:64, 0:256]), [bpat], [b_Al])
                    Z, b_Z = nxt(z_b)
                    Z3 = Z.rearrange("p (h n) -> p h n", h=4)
                    for h in range(4):
                        V(lambda e, Z3=Z3, vtok=vtok, h=h, bt=bt: e.tensor_scalar(out=Z3[0:64, h, 0:128], in0=vtok[0:64, h * 128:(h + 1) * 128], scalar1=bt[:, h:h + 1], scalar2=None, op0=ALU.mult),
                          [b_vtok, b_abt], [b_Z])
                        G_(lambda e, Z3=Z3, ktok=ktok, h=h, bt=bt, gsm=gsm: e.tensor_scalar(out=Z3[0:64, h, 128:256], in0=ktok[0:64, h * 128:(h + 1) * 128], scalar1=bt[:, h:h + 1],
                                                                                         scalar2=gsm[0:64, 4 + h:5 + h], op0=ALU.mult, op1=ALU.mult),
                           [b_ktok, b_abt, b_gsm], [b_Z])
                    cur, b_cur = Al, b_Al
                    for lvl in range(6):
                        pz0, bpz0 = self.ps(); pz1, bpz1 = self.ps()

                        def app(e, pz0=pz0, pz1=pz1, cur=cur, Z3=Z3):
                            ins = None
                            for h in range(4):
                                pz = pz0 if h < 2 else pz1
                                ins = e.matmul(pz[0:64, (h % 2) * 256:(h % 2) * 256 + 256], lhsT=cur[0:64, 256 + h * 64:256 + (h + 1) * 64], rhs=Z3[0:64, h, :], start=True, stop=True)
                            return ins
                        T_(app, [b_cur, b_Z], [bpz0, bpz1])
                        Zn, b_Zn = nxt(z_b)
                        op = ALU.subtract if lvl == 0 else ALU.add
                        V(lambda e, Zn=Zn, Z=Z, pz0=pz0, op=op: e.tensor_tensor(out=Zn[0:64, 0:512], in0=Z[0:64, 0:512], in1=pz0[0:64, :], op=op), [b_Z, bpz0], [b_Zn])
                        V(lambda e, Zn=Zn, Z=Z, pz1=pz1, op=op: e.tensor_tensor(out=Zn[0:64, 512:1024], in0=Z[0:64, 512:1024], in1=pz1[0:64, :], op=op), [b_Z, bpz1], [b_Zn])
                        Z, b_Z = Zn, b_Zn
                        Z3 = Z.rearrange("p (h n) -> p h n", h=4)
                        if lvl < 5:
                            psq, bpsq = self.ps()

                            def sq(e, psq=psq, cur=cur, lvl=lvl):
                                ins = None
                                for h in range(4):
                                    a = cur[0:64, h * 64:(h + 1) * 64]
                                    at = cur[0:64, 256 + h * 64:256 + (h + 1) * 64]
                                    if lvl < 4:
                                        e.matmul(psq[0:64, h * 64:(h + 1) * 64], lhsT=at, rhs=a, start=True, stop=True)
                                    ins = e.matmul(psq[0:64, 256 + h * 64:256 + (h + 1) * 64], lhsT=a, rhs=at, start=True, stop=True)
                                return ins
                            T_(sq, [b_cur], [bpsq])
                            nx, b_nx = nxt(A_b)
                            if lvl < 4:
                                S_(lambda e, nx=nx, psq=psq: e.copy(out=nx[0:64, :], in_=psq[0:64, :]), [bpsq], [b_nx])
                            else:
                                S_(lambda e, nx=nx, psq=psq: e.copy(out=nx[0:64, 256:512], in_=psq[0:64, 256:512]), [bpsq], [b_nx])
                            cur, b_cur = nx, b_nx
                    pw, bpw = self.ps()

                    def trw(e, pw=pw, Z3=Z3):
                        ins = None
                        for h in range(4):
                            ins = e.transpose(out=pw[:, h * 64:(h + 1) * 64], in_=Z3[0:64, h, 128:256], identity=idf[0:64, 0:64])
                        return ins
                    T_(trw, [b_Z, b_idf], [bpw])
                    wT, b_wT = nxt(wT_b)
                    S_(lambda e, wT=wT, pw=pw: e.copy(out=wT, in_=pw[:, 0:256]), [bpw], [b_wT])
                    qg, b_qg = nxt(qg_b); kd, b_kd = nxt(kd_b)
                    for h in range(4):
                        G_(lambda e, qg=qg, h=h, egbc=egbc, cs=cs, q3=q3: e.tensor_tensor(out=qg[:, h * 64:(h + 1) * 64], in0=q3[:, h, cs], in1=egbc[:, h * 64:(h + 1) * 64], op=ALU.mult),
                           [b_qkv, b_egbc], [b_qg])
                        G_(lambda e, kd=kd, h=h, ktok=ktok, gsm=gsm: e.tensor_scalar(out=kd[0:64, h * 128:(h + 1) * 128], in0=ktok[0:64, h * 128:(h + 1) * 128],
                                                                                  scalar1=gsm[0:64, 8 + h:9 + h], scalar2=None, op0=ALU.mult),
                           [b_ktok, b_gsm], [b_kd])
                    pu, bpu = self.ps()

                    def umm(e, pu=pu, wT=wT):
                        ins = None
                        for h in range(4):
                            ins = e.matmul(pu[0:64, h * 128:(h + 1) * 128], lhsT=wT[:, h * 64:(h + 1) * 64], rhs=S3[:, h, :], start=True, stop=True)
                        return ins
                    T_(umm, [b_wT, b_S], [bpu])
                    u, b_u = nxt(u_b)
                    u3 = u.rearrange("p (h n) -> p h n", h=4)
                    V(lambda e, u3=u3, Z3=Z3, pu=pu: e.tensor_tensor(out=u3[0:64, :, :], in0=Z3[0:64, :, 0:128], in1=pu[0:64, :].rearrange("p (h n) -> p h n", h=4), op=ALU.subtract),
                      [b_Z, bpu], [b_u])
                    if sm is None and sc == 1 and ci == 0:
                        self.dump("d_ab", abt[0:64, :], [b_abt], [64, 64])
                        self.dump("d_k0", q3[:, 4, 0:64], [b_qkv], [128, 64])
                        self.dump("d_gsm", gsm[0:64, :], [b_gsm], [64, 16])
                        self.dump("d_A", Al[0:64, :], [b_Al], [64, 512])
                        self.dump("d_Z", Z[0:64, :], [b_Z], [64, 1024])
                        self.dump("d_u", u[0:64, :], [b_u], [64, 512])
                        self.dump("d_S", St, [b_S], [128, 512])
                    po, bpo = self.ps()

                    def omm(e, po=po, qg=qg, aqk=aqk, u3=u3):
                        ins = None
                        for h in range(4):
                            e.matmul(po[0:64, h * 128:(h + 1) * 128], lhsT=qg[:, h * 64:(h + 1) * 64], rhs=S3[:, h, :], start=True, stop=False)
                            ins = e.matmul(po[0:64, h * 128:(h + 1) * 128], lhsT=aqk[0:64, h * 64:(h + 1) * 64], rhs=u3[0:64, h, :], start=False, stop=True)
                        return ins
                    T_(omm, [b_qg, b_aqk, b_u, b_S], [bpo])
                    pS, bpS = self.ps()

                    def smm(e, pS=pS, kd=kd, u3=u3):
                        ins = None
                        for h in range(4):
                            ins = e.matmul(pS[:, h * 128:(h + 1) * 128], lhsT=kd[0:64, h * 128:(h + 1) * 128], rhs=u3[0:64, h, :], start=True, stop=True)
                        return ins
                    T_(smm, [b_kd, b_u], [bpS])
                    for h in range(4):
                        V(lambda e, h=h, gc=gc, pS=pS: e.scalar_tensor_tensor(out=S3[:, h, :], in0=S3[:, h, :], scalar=gc[:, h:h + 1], in1=pS[:, h * 128:(h + 1) * 128],
                                                                            op0=ALU.mult, op1=ALU.add),
                          [b_S, b_gc, bpS], [b_S])
                    o, b_o = nxt(o_b); ob, b_ob = nxt(ob_b); stt, b_st = nxt(st_b)
                    G_(lambda e, stt=stt: e.memset(stt[0:64, 0:4], 0.0), [], [b_st])
                    for h in range(4):
                        S_(lambda e, o=o, po=po, stt=stt, h=h: e.activation(out=o[0:64, h * 128:(h + 1) * 128], in_=po[0:64, h * 128:(h + 1) * 128], func=AF.Square,
                                                                          accum_out=stt[0:64, h:h + 1]), [bpo, b_st], [b_o, b_st])
                    S_(lambda e, stt=stt: e.activation(out=stt[0:64, 4:8], in_=stt[0:64, 0:4], func=AF.Sqrt, bias=self.epsc[0:64, :], scale=1.0 / 128), [b_st, self.b_epsc], [b_st])
                    V(lambda e, stt=stt: e.reciprocal(out=stt[0:64, 4:8], in_=stt[0:64, 4:8]), [b_st], [b_st])
                    for h in range(4):
                        V(lambda e, o=o, po=po, stt=stt, h=h: e.scalar_tensor_tensor(out=o[0:64, h * 128:(h + 1) * 128], in0=po[0:64, h * 128:(h + 1) * 128],
                                                                                   scalar=stt[0:64, 4 + h:5 + h], in1=gnw[0:64, :], op0=ALU.mult, op1=ALU.mult),
                          [bpo, b_st, b_gnw, b_o], [b_o])
                    G_(lambda e, ob=ob, o=o, ci=ci, zs3=zs3: e.tensor_tensor(out=ob[0:64, :], in0=o[0:64, :], in1=zs3[0:64, ci, :], op=ALU.mult), [b_o, b_zs], [b_ob])
                    if sm is None and sc == 1 and ci == 0:
                        self.dump("d_o", o[0:64, :], [b_o], [64, 512])
                        self.dump("d_st", stt[0:64, :], [b_st], [64, 8])
                        self.dump("d_zs", zs[0:64, 0:512], [b_zs], [64, 512])
                    pt, bpt = self.ps()
                    ptb = pt[:, :].bitcast(BF16)

                    def tro(e, ptb=ptb, ob=ob):
                        ins = None
                        for h in range(4):
                            ins = e.transpose(out=ptb[:, h * 64:(h + 1) * 64], in_=ob[0:64, h * 128:(h + 1) * 128], identity=self.identb[0:64, 0:64])
                        return ins
                    T_(tro, [b_ob, self.b_identb], [bpt])
                    S_(lambda e, ptb=ptb, ci=ci, mix3=mix3: e.copy(out=mix3[:, :, ci * 64:(ci + 1) * 64], in_=ptb[:, 0:256].rearrange("p (h n) -> p h n", h=4)), [bpt], [b_mix])
                for h in range(4):
                    P.dma("sync", self.MIX_T[h * 128:(h + 1) * 128, c0:c0 + Wc], mix3[:, h, 0:Wc], reads=[b_mix])
            dst = self.o_pgdn if sm is None else self.o_sgdn[sm]
            P.dma("sync", dst.rearrange("h d e -> d h e") if sm is None else dst.rearrange("h d e -> d h e"), S3, reads=[b_S])
        A.release(m0)

    def phase_nsa(self):
        P, A = self.P, self.A
        self.ps_n = 6
        m0 = A.mark()
        V = lambda fn, r, w: P.add("vector", fn, r, w)
        S_ = lambda fn, r, w: P.add("scalar", fn, r, w)
        G_ = lambda fn, r, w: P.add("gpsimd", fn, r, w)
        T_ = lambda fn, r, w: P.add("tensor", fn, r, w)
        idf, b_idf = self.identf, self.b_identf
        idb, b_idb = self.identb, self.b_identb
        rb = A.alloc(8); b_rb = P.buf(); io = A.alloc(1); b_io = P.buf()
        P.dma("sync", rb[0:33, :], self.relb33.ap(), writes=[b_rb])
        P.dma("sync", io[0:33, :], self.iota33.ap(), writes=[b_io])
        m1 = A.mark()
        ixb = [(A.alloc(2048), P.buf()) for _ in range(2)]
        ohb = [(A.alloc(2048), P.buf()) for _ in range(2)]
        tbo = [(A.alloc(2048), P.buf()) for _ in range(2)]
        tabs = [(self.idx_sel, dd, 16384) for dd in range(14)] + [(self.idx_win, dd, 16384) for dd in range(5)] + [(self.idx_cmp, 0, 32768)]
        it = 0
        for ti, (src, row, n) in enumerate(tabs):
            for c in range(n // 2048):
                ix, b_ix = ixb[it % 2]; oh, b_oh = ohb[it % 2]; tb, b_tb = tbo[it % 2]
                it += 1
                P.dma("sync", ix[0:33, :], bass.AP(src, row * n + c * 2048, [[0, 33], [1, 2048]]), writes=[b_ix])
                V(lambda e, oh=oh, ix=ix: e.tensor_scalar(out=oh[0:33, :], in0=ix[0:33, :], scalar1=io[0:33, :], scalar2=None, op0=ALU.is_equal),
                  [b_ix, b_io], [b_oh])
                for j in range(4):
                    pt, bp = self.ps()
                    T_(lambda e, pt=pt, oh=oh, j=j: e.matmul(pt[0:8, :], lhsT=rb[0:33, :], rhs=oh[0:33, j * 512:(j + 1) * 512], start=True, stop=True),
                       [b_rb, b_oh], [bp])
                    S_(lambda e, tb=tb, pt=pt, j=j: e.copy(out=tb[0:8, j * 512:(j + 1) * 512], in_=pt[0:8, :]), [bp], [b_tb])
                P.dma("sync", self.TAB[ti, :, c * 2048:(c + 1) * 2048], tb[0:8, :], reads=[b_tb])
        A.release(m1)
        P.barrier()
        TselT = A.alloc(14 * 1024, BF16); b_TselT = P.buf()
        TwinT = A.alloc(5 * 1024, BF16); b_TwinT = P.buf()
        Ts3 = TselT.rearrange("p (d h q) -> p d h q", d=14, h=8)
        Tw3 = TwinT.rearrange("p (d h q) -> p d h q", d=5, h=8)
        for dd in range(14):
            P.dma("gpsimd", Ts3[:, dd, :, :], self.TAB[dd, :, 0:16384].rearrange("h (k q) -> k h q", q=128), writes=[b_TselT])
        for dd in range(5):
            P.dma("gpsimd", Tw3[:, dd, :, :], self.TAB[14 + dd, :, 0:16384].rearrange("h (k q) -> k h q", q=128), writes=[b_TwinT])
        TC = A.alloc(8 * 256); b_TC = P.buf()
        TC3 = TC.rearrange("p (h r) -> p h r", h=8)
        P.dma("sync", TC3, self.TAB[19, :, :].rearrange("h (q r) -> q h r", r=256), writes=[b_TC])
        wide = A.alloc(128 * 65, BF16); b_wide = P.buf()
        P.dma("gpsimd", wide, self.wide.ap(), writes=[b_wide])
        zt = A.alloc(260, BF16); b_zt = P.buf()
        G_(lambda e: e.memset(zt, 0.0), [], [b_zt])
        vmfa = A.alloc(512); b_vmfa = P.buf()
        P.dma("sync", vmfa, self.vmfa.ap(), writes=[b_vmfa])
        NKT = 65
        kselT = A.alloc(SEQ + 128, BF16); b_ksel = P.buf()
        kwinT = A.alloc(SEQ + 128, BF16); b_kwin = P.buf()
        P.dma("gpsimd", kselT[:, 0:SEQ], self.KF_T[2, :, 0:SEQ], writes=[b_ksel])
        P.dma("gpsimd", kwinT[:, 0:SEQ], self.KF_T[3, :, 0:SEQ], writes=[b_kwin])
        vsel = A.alloc(NKT * 130, BF16); b_vsel = P.buf()
        vwin = A.alloc(NKT * 130, BF16); b_vwin = P.buf()
        vs4 = vsel.rearrange("p (t g c) -> p t g c", t=NKT, g=2)
        vw4 = vwin.rearrange("p (t g c) -> p t g c", t=NKT, g=2)
        G_(lambda e: e.memset(vsel, 1.0), [], [b_vsel])
        G_(lambda e: e.memset(vwin, 1.0), [], [b_vwin])
        kvv = self.KV_TOK[0:SEQ, :].rearrange("(t p) n -> p t n", p=128)
        for g in range(2):
            P.dma("gpsimd", vs4[:, 0:64, g, 0:64], kvv[:, :, 384 + g * 64:384 + g * 64 + 64], writes=[b_vsel])
            P.dma("gpsimd", vw4[:, 0:64, g, 0:64], kvv[:, :, 640 + g * 64:640 + g * 64 + 64], writes=[b_vwin])
        kcT = A.alloc(128, BF16); b_kcT = P.buf()
        vcT = A.alloc(128); b_vcT = P.buf()
        vc = A.alloc(128, BF16); b_vc = P.buf()
        tmpm = A.alloc(128); b_tmpm = P.buf()
        m2 = A.mark()
        kcf = A.alloc(SEQ); b_kcf = P.buf()
        P.dma("sync", kcf, self.KF_T[0, :, 0:SEQ], writes=[b_kcf])
        V(lambda e: e.tensor_reduce(out=tmpm, in_=kcf.rearrange("p (n c) -> p n c", c=64), axis=AX.X, op=ALU.add), [b_kcf], [b_tmpm])
        S_(lambda e: e.mul(out=kcT, in_=tmpm, mul=1.0 / 64), [b_tmpm], [b_kcT])
        P.dma("sync", kcf, self.KF_T[1, :, 0:SEQ], reads=[b_tmpm], writes=[b_kcf])
        V(lambda e: e.tensor_reduce(out=vcT, in_=kcf.rearrange("p (n c) -> p n c", c=64), axis=AX.X, op=ALU.add), [b_kcf], [b_vcT])
        pt, bp = self.ps()
        T_(lambda e, pt=pt: e.transpose(out=pt[:, 0:128], in_=vcT, identity=idf), [b_vcT, b_idf], [bp])
        S_(lambda e, pt=pt: e.mul(out=vc, in_=pt[:, 0:128], mul=1.0 / 64), [bp], [b_vc])
        P.barrier()
        A.release(m2)
        def rot(n, cols, dt=F32):
            return [(A.alloc(cols, dt), P.buf()) for _ in range(n)]
        qb_b = rot(2, 512, BF16); gt_b = rot(2, 24)
        sc_b = rot(2, 512); pc_b = rot(2, 512); pcb_b = rot(2, 512, BF16); pcT_b = rot(2, 512, BF16)
        st_b = rot(2, 16); imp_b = rot(2, 128); mx_b = rot(2, 16); wk_b = rot(2, 128)
        ngm_b = rot(2, 128); nsT_b = rot(2, 512, BF16)
        PT_b = rot(3, 512, BF16)
        oc_b = rot(2, 256); on_b = rot(2, 260); ow_b = rot(2, 260)
        cf_b = rot(2, 24); ob_b = rot(2, 512, BF16); obT_b = rot(2, 512, BF16)
        ucnt = [0]

        def qblock(i, q0, Wq, samp):
            qb, b_qb = qb_b[i % 2]
            qb3 = qb[:, 0:4 * Wq].rearrange("p (t q) -> p t q", t=4)
            gt, b_gt = gt_b[i % 2]
            P.dma("sync", qb3, self.QN_T[:, :, q0:q0 + Wq].rearrange("t p q -> p t q"), writes=[b_qb])
            P.dma("sync", gt[0:Wq, :], self.GT[q0:q0 + Wq, :], writes=[b_gt])
            ob, b_ob = ob_b[i % 2]
            r0 = 128 - 2 * i
            thr_col = 14 if samp else 15
            for g in range(2):
                gs = slice(g * 64, g * 64 + 64)
                ucnt[0] += 1
                u = ucnt[0]
                pcs, bpcs = self.ps()

                def cmm(e, pcs=pcs, qb3=qb3, gs=gs):
                    ins = None
                    for h in range(4):
                        ins = e.matmul(pcs[0:Wq, h * 128:(h + 1) * 128], lhsT=qb3[gs, h, :], rhs=kcT[gs, :], start=True, stop=True)
                    return ins
                T_(cmm, [b_qb, b_kcT], [bpcs])
                sc, b_sc = sc_b[u % 2]; pc, b_pc = pc_b[u % 2]; stt, b_st = st_b[u % 2]
                V(lambda e, sc=sc, pcs=pcs, g=g: e.tensor_tensor(out=sc[0:Wq, :].rearrange("p (h n) -> p h n", h=4), in0=pcs[0:Wq, :].rearrange("p (h n) -> p h n", h=4),
                                                             in1=TC3[0:Wq, g * 4:g * 4 + 4, r0:r0 + 128], op=ALU.add), [bpcs, b_TC], [b_sc])
                G_(lambda e, stt=stt: e.memset(stt, 0.0), [], [b_st])
                for h in range(4):
                    S_(lambda e, pc=pc, sc=sc, stt=stt, h=h: e.activation(out=pc[0:Wq, h * 128:(h + 1) * 128], in_=sc[0:Wq, h * 128:(h + 1) * 128], func=AF.Exp,
                                                                      accum_out=stt[0:Wq, h:h + 1]), [b_sc, b_st], [b_pc, b_st])
                V(lambda e, stt=stt: e.tensor_scalar(out=stt[0:Wq, 4:8], in0=stt[0:Wq, 0:4], scalar1=1e-30, scalar2=None, op0=ALU.max), [b_st], [b_st])
                V(lambda e, stt=stt: e.reciprocal(out=stt[0:Wq, 4:8], in_=stt[0:Wq, 4:8]), [b_st], [b_st])
                imp, b_imp = imp_b[u % 2]
                V(lambda e, imp=imp, pc=pc, stt=stt: e.tensor_scalar(out=imp[0:Wq, :], in0=pc[0:Wq, 0:128], scalar1=stt[0:Wq, 4:5], scalar2=None, op0=ALU.mult), [b_pc, b_st], [b_imp])
                for h in range(1, 4):
                    V(lambda e, imp=imp, pc=pc, stt=stt, h=h: e.scalar_tensor_tensor(out=imp[0:Wq, :], in0=pc[0:Wq, h * 128:(h + 1) * 128], scalar=stt[0:Wq, 4 + h:5 + h], in1=imp[0:Wq, :],
                                                                                   op0=ALU.mult, op1=ALU.add), [b_pc, b_st, b_imp], [b_imp])
                V(lambda e, imp=imp: e.tensor_tensor(out=imp[0:Wq, :], in0=imp[0:Wq, :], in1=vmfa[0:Wq, r0:r0 + 128], op=ALU.mult), [b_imp, b_vmfa], [b_imp])
                V(lambda e, imp=imp: e.tensor_tensor(out=imp[0:Wq, :], in0=imp[0:Wq, :], in1=vmfa[0:Wq, 256 + r0:256 + r0 + 128], op=ALU.add), [b_imp, b_vmfa], [b_imp])
                G_(lambda e, imp=imp: e.memset(imp[0:Wq, 0:1], 1.0e4), [b_imp], [b_imp])
                mx, b_mx = mx_b[u % 2]; wk, b_wk = wk_b[u % 2]
                V(lambda e, mx=mx, imp=imp: e.max(out=mx[0:Wq, 0:8], in_=imp[0:Wq, :]), [b_imp], [b_mx])
                V(lambda e, mx=mx, imp=imp, wk=wk: e.match_replace(out=wk[0:Wq, :], in_to_replace=mx[0:Wq, 0:8], in_values=imp[0:Wq, :], imm_value=-1e30), [b_imp, b_mx], [b_wk])
                V(lambda e, mx=mx, wk=wk: e.max(out=mx[0:Wq, 8:16], in_=wk[0:Wq, :]), [b_wk, b_mx], [b_mx])
                ngm, b_ngm = ngm_b[u % 2]
                V(lambda e, ngm=ngm, imp=imp, mx=mx: e.tensor_scalar(out=ngm[0:Wq, :], in0=imp[0:Wq, :], scalar1=mx[0:Wq, thr_col:thr_col + 1], scalar2=None, op0=ALU.is_ge), [b_imp, b_mx], [b_ngm])
                V(lambda e, ngm=ngm: e.tensor_scalar(out=ngm[0:Wq, :], in0=ngm[0:Wq, :], scalar1=-1.0, scalar2=30000.0, op0=ALU.add, op1=ALU.mult), [b_ngm], [b_ngm])
                pnt, bpnt = self.ps()
                T_(lambda e, pnt=pnt, ngm=ngm: e.transpose(out=pnt[:, 0:Wq], in_=ngm[0:Wq, :], identity=idf[0:Wq, 0:Wq]), [b_ngm, b_idf], [bpnt])
                nsT, b_nsT = nsT_b[u % 2]
                for h in range(4):
                    (S_ if h % 2 == 0 else V)(lambda e, nsT=nsT, pnt=pnt, h=h: (e.copy if hasattr(e, "copy") else e.tensor_copy)(out=nsT[:, h * Wq:(h + 1) * Wq], in_=pnt[:, 0:Wq]),
                                              [bpnt], [b_nsT])
                pcb, b_pcb = pcb_b[u % 2]; pcT, b_pcT = pcT_b[u % 2]
                G_(lambda e, pcb=pcb, pc=pc: e.tensor_copy(out=pcb[0:Wq, :], in_=pc[0:Wq, :]), [b_pc], [b_pcb])
                ptt, bptt = self.ps()
                pttb = ptt[:, :].bitcast(BF16)

                def trp(e, pttb=pttb, pcb=pcb):
                    ins = None
                    for h in range(4):
                        ins = e.transpose(out=pttb[:, h * Wq:(h + 1) * Wq], in_=pcb[0:Wq, h * 128:(h + 1) * 128], identity=idb[0:Wq, 0:Wq])
                    return ins
                T_(trp, [b_pcb, b_idb], [bptt])
                S_(lambda e, pcT=pcT, pttb=pttb: e.copy(out=pcT[:, 0:4 * Wq], in_=pttb[:, 0:4 * Wq]), [bptt], [b_pcT])
                poc, bpoc = self.ps()

                def ocmm(e, poc=poc, pcT=pcT, gs=gs):
                    ins = None
                    for h in range(4):
                        ins = e.matmul(poc[0:Wq, h * 64:(h + 1) * 64], lhsT=pcT[:, h * Wq:(h + 1) * Wq], rhs=vc[:, gs], start=True, stop=True)
                    return ins
                T_(ocmm, [b_pcT, b_vc], [bpoc])
                oc, b_oc = oc_b[u % 2]
                S_(lambda e, oc=oc, poc=poc: e.copy(out=oc[0:Wq, :], in_=poc[0:Wq, 0:256]), [bpoc], [b_oc])
                pos, bpos = self.psum[6]
                pow_, bpow = self.psum[7]
                qrhs = qb3[gs, :, :]
                for br in range(2):
                    tlist = list(range(0, i + 1)) if br == 0 else list(range(max(0, i - 4), i + 1))
                    pacc, bpacc = (pos, bpos) if br == 0 else (pow_, bpow)
                    for t in tlist:
                        dd = i - t
                        tab = Ts3[:, min(dd, 13), g * 4:g * 4 + 4, 0:Wq] if br == 0 else Tw3[:, dd, g * 4:g * 4 + 4, 0:Wq]
                        btab = b_TselT if br == 0 else b_TwinT
                        kT = kselT if br == 0 else kwinT
                        bkT = b_ksel if br == 0 else b_kwin
                        va = vs4 if br == 0 else vw4
                        bva = b_vsel if br == 0 else b_vwin
                        use_mask = (br == 0 and t < 64)
                        pss, bpss = self.ps()

                        def smm(e, pss=pss, tab=tab, kT=kT, t=t, use_mask=use_mask, nsT=nsT, qrhs=qrhs, gs=gs):
                            e.matmul(pss[:, 0:4 * Wq], lhsT=idb, rhs=tab, start=True, stop=False)
                            if use_mask:
                                e.matmul(pss[:, 0:4 * Wq], lhsT=wide[:, t * 128:(t + 1) * 128], rhs=nsT[:, 0:4 * Wq], start=False, stop=False)
                            return e.matmul(pss[:, 0:4 * Wq], lhsT=kT[gs, t * 128:(t + 1) * 128], rhs=qrhs, start=False, stop=True)
                        T_(smm, [btab, bkT, b_qb, b_idb] + ([b_nsT, b_wide] if use_mask else []), [bpss])
                        PT, b_PT = PT_b[(t + br) % 3]
                        S_(lambda e, PT=PT, pss=pss: e.activation(out=PT[:, 0:4 * Wq], in_=pss[:, 0:4 * Wq], func=AF.Exp), [bpss], [b_PT])

                        def pvmm(e, pacc=pacc, PT=PT, va=va, t=t, g=g, first=(t == tlist[0]), last=(t == tlist[-1])):
                            ins = None
                            if first:
                                e.matmul(pacc[0:Wq, 0:260], lhsT=zt[:, 0:Wq], rhs=zt[:, 0:260], start=True, stop=False)
                            for h in range(4):
                                ins = e.matmul(pacc[0:Wq, h * 65:(h + 1) * 65], lhsT=PT[:, h * Wq:(h + 1) * Wq], rhs=va[:, t, g, :], start=False, stop=last)
                            return ins
                        T_(pvmm, [b_PT, bva, b_zt], [bpacc])
                on, b_on = on_b[u % 2]; ow, b_ow = ow_b[u % 2]
                S_(lambda e, on=on, pos=pos: e.copy(out=on[0:Wq, :], in_=pos[0:Wq, 0:260]), [bpos], [b_on])
                V(lambda e, ow=ow, pow_=pow_: e.tensor_copy(out=ow[0:Wq, :], in_=pow_[0:Wq, 0:260]), [bpow], [b_ow])
                cf, b_cf = cf_b[u % 2]
                on3 = on.rearrange("p (h c) -> p h c", h=4); ow3 = ow.rearrange("p (h c) -> p h c", h=4)
                cf3 = cf[:, 0:12].rearrange("p (h j) -> p h j", j=3)
                V(lambda e, cf3=cf3, stt=stt: e.tensor_copy(out=cf3[0:Wq, :, 0], in_=stt[0:Wq, 4:8]), [b_st], [b_cf])
                V(lambda e, cf3=cf3, on3=on3: e.reciprocal(out=cf3[0:Wq, :, 1], in_=on3[0:Wq, :, 64]), [b_on, b_cf], [b_cf])
                V(lambda e, cf3=cf3, ow3=ow3: e.reciprocal(out=cf3[0:Wq, :, 2], in_=ow3[0:Wq, :, 64]), [b_ow, b_cf], [b_cf])
                V(lambda e, cf=cf, gt=gt, g=g: e.tensor_tensor(out=cf[0:Wq, 0:12], in0=cf[0:Wq, 0:12], in1=gt[0:Wq, g * 12:g * 12 + 12], op=ALU.mult), [b_cf, b_gt], [b_cf])
                for h in range(4):
                    V(lambda e, oc=oc, cf=cf, h=h: e.tensor_scalar(out=oc[0:Wq, h * 64:(h + 1) * 64], in0=oc[0:Wq, h * 64:(h + 1) * 64], scalar1=cf[0:Wq, 3 * h:3 * h + 1], scalar2=None, op0=ALU.mult),
                      [b_oc, b_cf], [b_oc])
                    V(lambda e, oc=oc, on3=on3, cf=cf, h=h: e.scalar_tensor_tensor(out=oc[0:Wq, h * 64:(h + 1) * 64], in0=on3[0:Wq, h, 0:64], scalar=cf[0:Wq, 3 * h + 1:3 * h + 2],
                                                                                 in1=oc[0:Wq, h * 64:(h + 1) * 64], op0=ALU.mult, op1=ALU.add), [b_oc, b_on, b_cf], [b_oc])
                    V(lambda e, oc=oc, ow3=ow3, cf=cf, h=h, ob=ob, g=g: e.scalar_tensor_tensor(out=ob[0:Wq, g * 256 + h * 64:g * 256 + (h + 1) * 64], in0=ow3[0:Wq, h, 0:64],
                                                                                           scalar=cf[0:Wq, 3 * h + 2:3 * h + 3], in1=oc[0:Wq, h * 64:(h + 1) * 64],
                                                                                           op0=ALU.mult, op1=ALU.add), [b_oc, b_ow, b_cf], [b_ob])
            pto, bpto = self.ps()
            ptob = pto[:, :].bitcast(BF16)

            def tro(e, ptob=ptob, ob=ob):
                ins = None
                for k in range(4):
                    ins = e.transpose(out=ptob[:, k * Wq:(k + 1) * Wq], in_=ob[0:Wq, k * 128:(k + 1) * 128], identity=idb[0:Wq, 0:Wq])
                return ins
            T_(tro, [b_ob, b_idb], [bpto])
            obT, b_obT = obT_b[i % 2]
            S_(lambda e, obT=obT, ptob=ptob: e.copy(out=obT[:, 0:4 * Wq], in_=ptob[:, 0:4 * Wq]), [bpto], [b_obT])
            P.dma("sync", self.MIX_T[512:1024, q0:q0 + Wq].rearrange("(k p) q -> p k q", p=128), obT[:, 0:4 * Wq].rearrange("p (k q) -> p k q", k=4), reads=[b_obT])

        for i in range(SEQ // 128):
            qblock(i, i * 128, 128, False)
        if os.environ.get("NO_SAMP") is None:
            self.nsa_samples(qblock, dict(kselT=kselT, b_ksel=b_ksel, kwinT=kwinT, b_kwin=b_kwin, vs4=vs4, b_vsel=b_vsel, vw4=vw4, b_vwin=b_vwin,
                                          kcT=kcT, b_kcT=b_kcT, vc=vc, b_vc=b_vc))
        self.ps_n = 8
        A.release(m0)

    def nsa_samples(self, qblock, tb):
        P, A = self.P, self.A
        V = lambda fn, r, w: P.add("vector", fn, r, w)
        S_ = lambda fn, r, w: P.add("scalar", fn, r, w)
        G_ = lambda fn, r, w: P.add("gpsimd", fn, r, w)
        T_ = lambda fn, r, w: P.add("tensor", fn, r, w)
        idf, b_idf = self.identf, self.b_identf
        kselT, b_ksel, kwinT, b_kwin = tb["kselT"], tb["b_ksel"], tb["kwinT"], tb["b_kwin"]
        vs4, b_vsel, vw4, b_vwin = tb["vs4"], tb["b_vsel"], tb["vw4"], tb["b_vwin"]
        kcT, b_kcT, vc, b_vc = tb["kcT"], tb["b_kcT"], tb["vc"], tb["b_vc"]
        hw = A.alloc(256); b_hw = P.buf()
        P.dma("sync", hw, self.hwide.ap(), writes=[b_hw])
        iop = A.alloc(1); b_iop = P.buf()
        P.dma("sync", iop, self.iotap.ap(), writes=[b_iop])
        pti = A.alloc(64, I32); b_pti = P.buf()
        idx = A.alloc(64, I32); b_idx = P.buf()
        pgs = [(A.alloc(512), P.buf()) for _ in range(2)]
        wts_ = [(A.alloc(256), P.buf()) for _ in range(2)]
        pkc, bpkc = self.psum[6]
        pvc, bpvc = self.psum[7]
        for sm in range(NSAMP):
            col = SEQ + 64 * sm
            P.dma("sync", pti, bass.AP(self.ptab, sm * 64, [[0, 128], [1, 64]]), writes=[b_pti])
            V(lambda e: e.tensor_scalar(out=idx, in0=pti, scalar1=128.0, scalar2=iop[:, 0:1], op0=ALU.mult, op1=ALU.add), [b_pti, b_iop], [b_idx])
            G_(lambda e: e.memset(kselT[:, SEQ:SEQ + 128], 0.0), [], [b_ksel])
            G_(lambda e: e.memset(kwinT[:, SEQ:SEQ + 128], 0.0), [], [b_kwin])
            G_(lambda e: e.memset(vs4[:, 64, :, 0:64], 0.0), [], [b_vsel])
            G_(lambda e: e.memset(vw4[:, 64, :, 0:64], 0.0), [], [b_vwin])
            P.dma("gpsimd", kselT[:, SEQ:SEQ + 1], self.KF_T[2, :, col:col + 1], writes=[b_ksel], allow_slow_non_contiguous=True)
            P.dma("gpsimd", kwinT[:, SEQ:SEQ + 1], self.KF_T[3, :, col:col + 1], writes=[b_kwin], allow_slow_non_contiguous=True)
            for g in range(2):
                P.dma("gpsimd", vs4[0:1, 64, g, 0:64], self.KV_TOK[col:col + 1, 384 + g * 64:448 + g * 64], writes=[b_vsel])
                P.dma("gpsimd", vw4[0:1, 64, g, 0:64], self.KV_TOK[col:col + 1, 640 + g * 64:704 + g * 64], writes=[b_vwin])
            for sl in range(64):
                pg, b_pg = pgs[sl % 2]
                P.add("gpsimd", lambda e, pg=pg, sl=sl: e.indirect_dma_start(out=pg, out_offset=None, in_=self.cache2d[:, :],
                                                                           in_offset=bass.IndirectOffsetOnAxis(ap=idx[:, sl:sl + 1], axis=0)),
                      [b_idx], [b_pg], dma=True)
                pt, bp = self.ps()
                T_(lambda e, pt=pt, pg=pg: e.transpose(out=pt[:, 0:128], in_=pg[:, 256:384], identity=idf), [b_pg, b_idf], [bp])
                S_(lambda e, pt=pt, sl=sl: e.copy(out=kselT[:, sl * 128:(sl + 1) * 128], in_=pt[:, 0:128]), [bp], [b_ksel])
                V(lambda e, pg=pg, sl=sl: e.tensor_copy(out=vs4[:, sl, :, 0:64], in_=pg[:, 384:512].rearrange("p (g c) -> p g c", g=2)), [b_pg], [b_vsel])

                def mean_mm(e, pg=pg, sl=sl):
                    e.matmul(pkc[:, 2 * sl:2 * sl + 2], lhsT=pg[:, 0:128], rhs=hw[:, 126:128], start=True, stop=True)
                    return e.matmul(pvc[:, 0:128], lhsT=hw[:, 126 - 2 * sl:254 - 2 * sl], rhs=pg[:, 128:256], start=(sl == 0), stop=(sl == 63))
                T_(mean_mm, [b_pg, b_hw], [bpkc, bpvc])
            S_(lambda e: e.copy(out=kcT, in_=pkc[:, 0:128]), [bpkc], [b_kcT])
            V(lambda e: e.tensor_copy(out=vc, in_=pvc[:, 0:128]), [bpvc], [b_vc])
            for j in range(4):
                wt, b_wt = wts_[j % 2]
                P.dma("sync", wt, self.cwin[sm, j * 128:(j + 1) * 128, :], writes=[b_wt])
                pt, bp = self.ps()
                T_(lambda e, pt=pt, wt=wt: e.transpose(out=pt[:, 0:128], in_=wt[:, 0:128], identity=idf), [b_wt, b_idf], [bp])
                S_(lambda e, pt=pt, j=j: e.copy(out=kwinT[:, (60 + j) * 128:(61 + j) * 128], in_=pt[:, 0:128]), [bp], [b_kwin])
                V(lambda e, wt=wt, j=j: e.tensor_copy(out=vw4[:, 60 + j, :, 0:64], in_=wt[:, 128:256].rearrange("p (g c) -> p g c", g=2)), [b_wt], [b_vwin])
            qblock(64, col, 64, True)

    def load_w_bf16(self, dst3, src, nk, b_w):
        for k in range(nk):
            self.P.dma("gpsimd", dst3[:, k, :], src[k * 128:(k + 1) * 128, :], writes=[b_w])

    def phase_b1a(self):
        P, A = self.P, self.A
        m0 = A.mark()
        wo = A.alloc(8 * D, BF16).rearrange("p (k n) -> p k n", k=8); b_wo = P.buf()
        self.load_w_bf16(wo, self.w_out0, 8, b_wo)
        mtg = self.mod_tiles(0, 0, only_gate=True)
        mtf = self.mod_tiles(0, 1)
        xts = [(A.alloc(D), P.buf()) for _ in range(2)]
        x1s = [(A.alloc(D), P.buf()) for _ in range(2)]
        mxs = [(A.alloc(8 * 128, BF16).rearrange("p (k n) -> p k n", k=8), P.buf()) for _ in range(2)]
        hTs = [(A.alloc(8 * 128, BF16).rearrange("p (k n) -> p k n", k=8), P.buf()) for _ in range(2)]
        tmp = [(A.alloc(512), P.buf()) for _ in range(2)]
        scrs = [(A.alloc(D, BF16), P.buf(), A.alloc(1), P.buf(), A.alloc(1), P.buf(), A.alloc(D), P.buf(), A.alloc(D, BF16), P.buf()) for _ in range(2)]
        for t in range(NTILE):
            r0 = t * 128
            ty = self.tile_type(t)
            gate = mtg[ty][4]; b_gate = mtg[ty][5]
            xt, b_x = xts[t % 2]; x1, b_x1 = x1s[t % 2]; mx, b_mx = mxs[t % 2]; hT, b_hT = hTs[t % 2]
            P.dma("sync", xt, self.xall[r0:r0 + 128, :], writes=[b_x])
            P.dma("sync", mx, self.MIX_T[:, r0:r0 + 128].rearrange("(k p) n -> p k n", p=128), writes=[b_mx])
            for c in range(2):
                pt, bp = self.ps()

                def mm(e, pt=pt, mx=mx, c=c):
                    ins = None
                    for k in range(8):
                        ins = e.matmul(pt[:, :], lhsT=mx[:, k, :], rhs=wo[:, k, c * 512:(c + 1) * 512], start=(k == 0), stop=(k == 7))
                    return ins
                P.add("tensor", mm, [b_mx, b_wo], [bp])
                tm, b_tm = tmp[c]
                P.add("vector", lambda e, tm=tm, pt=pt, gate=gate, c=c: e.tensor_tensor(out=tm, in0=pt[:, :], in1=gate[:, c * 512:(c + 1) * 512], op=ALU.mult),
                      [bp, b_gate], [b_tm])
                P.add("gpsimd", lambda e, tm=tm, x1=x1, xt=xt, c=c: e.tensor_tensor(out=x1[:, c * 512:(c + 1) * 512], in0=tm, in1=xt[:, c * 512:(c + 1) * 512], op=ALU.add),
                      [b_tm, b_x], [b_x1])
            P.dma("sync", self.X1[r0:r0 + 128, :], x1, reads=[b_x1])
            self.norm_mod_transpose(x1, b_x1, mtf[ty], hT, b_hT, 0, scrs[t % 2])
            P.dma("sync", self.H2T[:, r0:r0 + 128].rearrange("(k p) n -> p k n", p=128), hT, reads=[b_hT])
        A.release(m0)

    def phase_b1b(self):
        P, A = self.P, self.A
        m0 = A.mark()
        NF = FFN // 128
        wg = A.alloc(8 * FFN, BF16).rearrange("p (k n) -> p k n", k=8); b_wg = P.buf()
        wu = A.alloc(8 * FFN, BF16).rearrange("p (k n) -> p k n", k=8); b_wu = P.buf()
        wd = A.alloc(NF * D, BF16).rearrange("p (k n) -> p k n", k=NF); b_wd = P.buf()
        self.load_w_bf16(wg, self.ffn_wg, 8, b_wg)
        self.load_w_bf16(wu, self.ffn_wu, 8, b_wu)
        self.load_w_bf16(wd, self.ffn_wd, NF, b_wd)
        mtg = self.mod_tiles(0, 1, only_gate=True)
        hTs = [(A.alloc(8 * 256, BF16).rearrange("p (k n) -> p k n", k=8), P.buf()) for _ in range(2)]
        acts = [(A.alloc(NF * 256, BF16).rearrange("p (k n) -> p k n", k=NF), P.buf()) for _ in range(1)]
        sgs = [(A.alloc(512), P.buf()) for _ in range(2)]
        x1s = [(A.alloc(D), P.buf()) for _ in range(2)]
        x2s = [(A.alloc(D), P.buf()) for _ in range(2)]
        tmp = [(A.alloc(512), P.buf()) for _ in range(2)]
        ti = 0
        for s_ in range(TT // 256):
            W = 256
            c0 = s_ * 256
            hT, b_hT = hTs[s_ % 2]
            act, b_act = acts[0]
            P.dma("sync", hT[:, :, 0:W], self.H2T[:, c0:c0 + W].rearrange("(k p) n -> p k n", p=128), writes=[b_hT])
            for ft in range(NF):
                pg, bpg = self.ps(); pu, bpu = self.ps()

                def mm(e, pg=pg, pu=pu, hT=hT, ft=ft, W=W):
                    ins = None
                    for k in range(8):
                        e.matmul(pg[:, 0:W], lhsT=wg[:, k, ft * 128:(ft + 1) * 128], rhs=hT[:, k, 0:W], start=(k == 0), stop=(k == 7))
                    for k in range(8):
                        ins = e.matmul(pu[:, 0:W], lhsT=wu[:, k, ft * 128:(ft + 1) * 128], rhs=hT[:, k, 0:W], start=(k == 0), stop=(k == 7))
                    return ins
                P.add("tensor", mm, [b_wg, b_wu, b_hT], [bpg, bpu])
                sg, b_sg = sgs[ft % 2]
                P.add("scalar", lambda e, sg=sg, pg=pg, W=W: e.activation(out=sg[:, 0:W], in_=pg[:, 0:W], func=AF.Silu), [bpg], [b_sg])
                P.add("vector", lambda e, act=act, sg=sg, pu=pu, ft=ft, W=W: e.tensor_tensor(out=act[:, ft, 0:W], in0=sg[:, 0:W], in1=pu[:, 0:W], op=ALU.mult),
                      [b_sg, bpu], [b_act])
            for tl in range(W // 128):
                t = s_ * 2 + tl
                r0 = t * 128
                ty = self.tile_type(t)
                gate = mtg[ty][4]; b_gate = mtg[ty][5]
                x1, b_x1 = x1s[ti % 2]; x2, b_x2 = x2s[ti % 2]
                ti += 1
                P.dma("sync", x1, self.X1[r0:r0 + 128, :], writes=[b_x1])
                for c in range(2):
                    pt, bp = self.ps()

                    def mm2(e, pt=pt, act=act, tl=tl, c=c):
                        ins = None
                        for ft in range(NF):
                            ins = e.matmul(pt[:, :], lhsT=act[:, ft, tl * 128:(tl + 1) * 128], rhs=wd[:, ft, c * 512:(c + 1) * 512], start=(ft == 0), stop=(ft == NF - 1))
                        return ins
                    P.add("tensor", mm2, [b_act, b_wd], [bp])
                    tm, b_tm = tmp[c]
                    P.add("vector", lambda e, tm=tm, pt=pt, gate=gate, c=c: e.tensor_tensor(out=tm, in0=pt[:, :], in1=gate[:, c * 512:(c + 1) * 512], op=ALU.mult),
                          [bp, b_gate], [b_tm])
                    P.add("gpsimd", lambda e, tm=tm, x2=x2, x1=x1, c=c: e.tensor_tensor(out=x2[:, c * 512:(c + 1) * 512], in0=tm, in1=x1[:, c * 512:(c + 1) * 512], op=ALU.add),
                          [b_tm, b_x1], [b_x2])
                P.dma("sync", self.X2[r0:r0 + 128, :], x2, reads=[b_x2])
        A.release(m0)

    def phase_b2(self):
        P, A = self.P, self.A
        m0 = A.mark()
        V = lambda fn, r, w: P.add("vector", fn, r, w)
        S_ = lambda fn, r, w: P.add("scalar", fn, r, w)
        G_ = lambda fn, r, w: P.add("gpsimd", fn, r, w)
        T_ = lambda fn, r, w: P.add("tensor", fn, r, w)
        wi = A.alloc(8 * 2 * D, BF16).rearrange("p (k n) -> p k n", k=8); b_wi = P.buf()
        wo = A.alloc(8 * D, BF16).rearrange("p (k n) -> p k n", k=8); b_wo = P.buf()
        self.load_w_bf16(wi, self.w_in1, 8, b_wi)
        self.load_w_bf16(wo, self.w_out1, 8, b_wo)
        wa = A.alloc(8 * 128).rearrange("p (n e) -> p n e", n=8); b_wa = P.buf()
        wx = A.alloc(8 * 128).rearrange("p (n e) -> p n e", n=8); b_wx = P.buf()
        P.dma("sync", wa, self.lru_wa.ap().rearrange("n d e -> d n e"), writes=[b_wa])
        P.dma("sync", wx, self.lru_wx.ap().rearrange("n d e -> d n e"), writes=[b_wx])
        lv = A.alloc(64); b_lv = P.buf()
        P.dma("sync", lv, self.lru_vec.ap(), writes=[b_lv])
        lv3 = lv.rearrange("p (n j) -> p n j", j=8)
        c1 = A.alloc(8); b_c1 = P.buf()
        S_(lambda e: e.activation(out=c1, in_=lv3[:, :, 7], func=AF.Exp, scale=-1.0), [b_lv], [b_c1])
        S_(lambda e: e.activation(out=c1, in_=c1, func=AF.Ln, bias=1.0), [b_c1], [b_c1])
        V(lambda e: e.tensor_scalar(out=c1, in0=c1, scalar1=-8.0, scalar2=None, op0=ALU.mult), [b_c1], [b_c1])
        mt = self.mod_tiles(1, 0, need_gate=True)
        xts = [(A.alloc(D), P.buf()) for _ in range(2)]
        scrs = [(A.alloc(D, BF16), P.buf(), A.alloc(1), P.buf(), A.alloc(1), P.buf(), A.alloc(D), P.buf(), A.alloc(D, BF16), P.buf()) for _ in range(2)]
        hTs = [(A.alloc(8 * 512, BF16).rearrange("p (k n) -> p k n", k=8), P.buf()) for _ in range(1)]
        xr = A.alloc(8 * 515).rearrange("p (n c) -> p n c", n=8); bxr = [P.buf() for _ in range(8)]
        hst = A.alloc(8); b_hst = P.buf()
        G_(lambda e: e.memset(xr[:, :, 0:3], 0.0), [], bxr)
        G_(lambda e: e.memset(hst, 0.0), [], [b_hst])
        hs0 = A.alloc(NSAMP * 8); b_hs0 = P.buf()
        for sm in range(NSAMP):
            P.dma("sync", hs0[:, sm * 8:(sm + 1) * 8], bass.AP(self.slru, sm * D, [[1, 128], [128, 8]]), writes=[b_hs0], allow_slow_non_contiguous=True)
        def rot(n, cols, dt=F32):
            return [(A.alloc(cols, dt), P.buf()) for _ in range(n)]
        xc_b = rot(2, 512); r_b = rot(1, 512); i_b = rot(1, 512); a_b = rot(1, 512); b_b = rot(1, 512); h_b = rot(1, 512)
        gg_b = rot(1, 512); g2_b = rot(1, 512)
        yT = A.alloc(8 * 512, BF16).rearrange("p (n c) -> p n c", n=8); b_yT = P.buf()
        x2s = [(A.alloc(D), P.buf()) for _ in range(2)]; x3s = [(A.alloc(D), P.buf()) for _ in range(2)]
        tmp = [(A.alloc(512), P.buf()) for _ in range(2)]
        ti = 0
        ui = 0
        for s_ in range(17):
            W = 512 if s_ < 16 else 256
            hT, b_hT = hTs[0]
            for tl in range(W // 128):
                t = s_ * 4 + tl
                xt, b_x = xts[ti % 2]; scr = scrs[ti % 2]
                ti += 1
                P.dma("sync", xt, self.X2[t * 128:(t + 1) * 128, :], writes=[b_x])
                self.norm_mod_transpose(xt, b_x, mt[self.tile_type(t)], hT, b_hT, tl * 128, scr)
            for n in range(8):
                ui += 1
                pr, bpr = self.ps(); pgt, bpgt = self.ps()

                def mm(e, pr=pr, pgt=pgt, hT=hT, n=n, W=W):
                    ins = None
                    for k in range(8):
                        e.matmul(pr[:, 0:W], lhsT=wi[:, k, D + n * 128:D + (n + 1) * 128], rhs=hT[:, k, 0:W], start=(k == 0), stop=(k == 7))
                    for k in range(8):
                        ins = e.matmul(pgt[:, 0:W], lhsT=wi[:, k, n * 128:(n + 1) * 128], rhs=hT[:, k, 0:W], start=(k == 0), stop=(k == 7))
                    return ins
                T_(mm, [b_wi, b_hT], [bpr, bpgt])
                if s_ == 16:
                    for sm in range(NSAMP):
                        src = bass.AP(self.slconv, sm * 3 * D + n * 128, [[1, 128], [D, 3]])
                        P.dma("sync", xr[:, n, 64 * sm:64 * sm + 3], src, writes=[bxr[n]], allow_slow_non_contiguous=True)
                        lo = 64 * sm + 3
                        hi = 64 * sm + 64 if sm < NSAMP - 1 else W + 3
                        S_(lambda e, pr=pr, n=n, lo=lo, hi=hi: e.copy(out=xr[:, n, lo:hi], in_=pr[:, lo - 3:hi - 3]), [bpr], [bxr[n]])
                else:
                    S_(lambda e, pr=pr, n=n, W=W: e.copy(out=xr[:, n, 3:3 + W], in_=pr[:, 0:W]), [bpr], [bxr[n]])
                xc, b_xc = xc_b[ui % 2]
                V(lambda e, xc=xc, n=n, W=W: e.tensor_scalar(out=xc[:, 0:W], in0=xr[:, n, 3:3 + W], scalar1=lv3[:, n, 3:4], scalar2=lv3[:, n, 4:5], op0=ALU.mult, op1=ALU.add),
                  [bxr[n], b_lv], [b_xc])
                for j in range(3):
                    V(lambda e, xc=xc, n=n, W=W, j=j: e.scalar_tensor_tensor(out=xc[:, 0:W], in0=xr[:, n, j:j + W], scalar=lv3[:, n, j:j + 1], in1=xc[:, 0:W], op0=ALU.mult, op1=ALU.add),
                      [bxr[n], b_lv, b_xc], [b_xc])
                if s_ == 15:
                    dst = bass.AP(self.o_plconv, n * 128, [[1, 128], [D, 3]])
                    P.dma("sync", dst, xr[:, n, W:W + 3], reads=[bxr[n]], allow_slow_non_contiguous=True)
                if s_ < 15:
                    G_(lambda e, n=n, W=W: e.tensor_copy(out=xr[:, n, 0:3], in_=xr[:, n, W:W + 3]), [bxr[n]], [bxr[n]])
                pa, bpa = self.ps(); px, bpx = self.ps()
                T_(lambda e, pa=pa, px=px, xc=xc, n=n, W=W: (e.matmul(pa[:, 0:W], lhsT=wa[:, n, :], rhs=xc[:, 0:W], start=True, stop=True),
                                                         e.matmul(px[:, 0:W], lhsT=wx[:, n, :], rhs=xc[:, 0:W], start=True, stop=True))[1],
                   [b_wa, b_wx, b_xc], [bpa, bpx])
                r_, b_r = r_b[0]; i_, b_i = i_b[0]; a_, b_a = a_b[0]; bb_, b_bb = b_b[0]; h_, b_h = h_b[0]
                S_(lambda e, r_=r_, pa=pa, n=n, W=W: e.activation(out=r_[:, 0:W], in_=pa[:, 0:W], func=AF.Sigmoid, bias=lv3[:, n, 5:6]), [bpa, b_lv], [b_r])
                S_(lambda e, i_=i_, px=px, n=n, W=W: e.activation(out=i_[:, 0:W], in_=px[:, 0:W], func=AF.Sigmoid, bias=lv3[:, n, 6:7]), [bpx, b_lv], [b_i])
                S_(lambda e, a_=a_, r_=r_, n=n, W=W: e.activation(out=a_[:, 0:W], in_=r_[:, 0:W], func=AF.Exp, scale=c1[:, n:n + 1]), [b_r, b_c1], [b_a])
                G_(lambda e, bb_=bb_, a_=a_, W=W: e.tensor_tensor(out=bb_[:, 0:W], in0=a_[:, 0:W], in1=a_[:, 0:W], op=ALU.mult), [b_a], [b_bb])
                V(lambda e, bb_=bb_, W=W: e.tensor_scalar(out=bb_[:, 0:W], in0=bb_[:, 0:W], scalar1=-1.0, scalar2=1.0, op0=ALU.mult, op1=ALU.add), [b_bb], [b_bb])
                V(lambda e, bb_=bb_, W=W: e.tensor_scalar(out=bb_[:, 0:W], in0=bb_[:, 0:W], scalar1=0.0, scalar2=None, op0=ALU.max), [b_bb], [b_bb])
                S_(lambda e, bb_=bb_, W=W: e.activation(out=bb_[:, 0:W], in_=bb_[:, 0:W], func=AF.Sqrt), [b_bb], [b_bb])
                G_(lambda e, i_=i_, xc=xc, W=W: e.tensor_tensor(out=i_[:, 0:W], in0=i_[:, 0:W], in1=xc[:, 0:W], op=ALU.mult), [b_i, b_xc], [b_i])
                V(lambda e, bb_=bb_, i_=i_, W=W: e.tensor_tensor(out=bb_[:, 0:W], in0=bb_[:, 0:W], in1=i_[:, 0:W], op=ALU.mult), [b_bb, b_i], [b_bb])
                if s_ == 16:
                    for sm in range(NSAMP):
                        V(lambda e, h_=h_, a_=a_, bb_=bb_, n=n, sm=sm: e.tensor_tensor_scan(out=h_[:, 64 * sm:64 * sm + 64], data0=a_[:, 64 * sm:64 * sm + 64], data1=bb_[:, 64 * sm:64 * sm + 64],
                                                                                         initial=hs0[:, sm * 8 + n:sm * 8 + n + 1], op0=ALU.mult, op1=ALU.add), [b_a, b_bb, b_hs0], [b_h])
                        P.dma("sync", bass.AP(self.o_slru, sm * D + n * 128, [[1, 128], [1, 1]]), h_[:, 64 * sm:64 * sm + 1], reads=[b_h], allow_slow_non_contiguous=True)
                        P.dma("sync", bass.AP(self.o_slconv, sm * 3 * D + n * 128, [[1, 128], [D, 3]]), xr[:, n, 64 * sm + 1:64 * sm + 4], reads=[bxr[n]], allow_slow_non_contiguous=True)
                else:
                    V(lambda e, h_=h_, a_=a_, bb_=bb_, n=n, W=W: e.tensor_tensor_scan(out=h_[:, 0:W], data0=a_[:, 0:W], data1=bb_[:, 0:W], initial=hst[:, n:n + 1],
                                                                                   op0=ALU.mult, op1=ALU.add), [b_a, b_bb, b_hst], [b_h])
                    G_(lambda e, h_=h_, n=n, W=W: e.tensor_copy(out=hst[:, n:n + 1], in_=h_[:, W - 1:W]), [b_h, b_hst], [b_hst])
                gg, b_gg = gg_b[0]; g2, b_g2 = g2_b[0]
                S_(lambda e, gg=gg, pgt=pgt, W=W: e.copy(out=gg[:, 0:W], in_=pgt[:, 0:W]), [bpgt], [b_gg])
                G_(lambda e, gg=gg, g2=g2, W=W: e.tensor_tensor(out=g2[:, 0:W], in0=gg[:, 0:W], in1=gg[:, 0:W], op=ALU.mult), [b_gg], [b_g2])
                V(lambda e, g2=g2, W=W: e.tensor_scalar(out=g2[:, 0:W], in0=g2[:, 0:W], scalar1=0.044715, scalar2=1.0, op0=ALU.mult, op1=ALU.add), [b_g2], [b_g2])
                G_(lambda e, gg=gg, g2=g2, W=W: e.tensor_tensor(out=g2[:, 0:W], in0=g2[:, 0:W], in1=gg[:, 0:W], op=ALU.mult), [b_gg, b_g2], [b_g2])
                S_(lambda e, g2=g2, W=W: e.activation(out=g2[:, 0:W], in_=g2[:, 0:W], func=AF.Sigmoid, scale=1.5957691216057308), [b_g2], [b_g2])
                G_(lambda e, gg=gg, g2=g2, W=W: e.tensor_tensor(out=g2[:, 0:W], in0=g2[:, 0:W], in1=gg[:, 0:W], op=ALU.mult), [b_gg, b_g2], [b_g2])
                V(lambda e, g2=g2, h_=h_, n=n, W=W: e.tensor_tensor(out=yT[:, n, 0:W], in0=g2[:, 0:W], in1=h_[:, 0:W], op=ALU.mult), [b_g2, b_h], [b_yT])
            if s_ == 15:
                P.dma("sync", bass.AP(self.o_plru, 0, [[1, 128], [128, 8]]), hst, reads=[b_hst], allow_slow_non_contiguous=True)
            for tl in range(W // 128):
                t = s_ * 4 + tl
                r0 = t * 128
                ty = self.tile_type(t)
                gate = mt[ty][4]; b_gate = mt[ty][5]
                x2, b_x2 = x2s[t % 2]; x3, b_x3 = x3s[t % 2]
                P.dma("sync", x2, self.X2[r0:r0 + 128, :], writes=[b_x2])
                for c in range(2):
                    pt, bp = self.ps()

                    def mm2(e, pt=pt, tl=tl, c=c):
                        ins = None
                        for n in range(8):
                            ins = e.matmul(pt[:, :], lhsT=yT[:, n, tl * 128:(tl + 1) * 128], rhs=wo[:, n, c * 512:(c + 1) * 512], start=(n == 0), stop=(n == 7))
                        return ins
                    T_(mm2, [b_yT, b_wo], [bp])
                    tm, b_tm = tmp[c]
                    V(lambda e, tm=tm, pt=pt, gate=gate, c=c: e.tensor_tensor(out=tm, in0=pt[:, :], in1=gate[:, c * 512:(c + 1) * 512], op=ALU.mult), [bp, b_gate], [b_tm])
                    G_(lambda e, tm=tm, x3=x3, x2=x2, c=c: e.tensor_tensor(out=x3[:, c * 512:(c + 1) * 512], in0=tm, in1=x2[:, c * 512:(c + 1) * 512], op=ALU.add), [b_tm, b_x2], [b_x3])
                P.dma("sync", self.X3[r0:r0 + 128, :], x3, reads=[b_x3])
        A.release(m0)

    def phase_c1(self):
        P, A = self.P, self.A
        m0 = A.mark()
        V = lambda fn, r, w: P.add("vector", fn, r, w)
        S_ = lambda fn, r, w: P.add("scalar", fn, r, w)
        G_ = lambda fn, r, w: P.add("gpsimd", fn, r, w)
        T_ = lambda fn, r, w: P.add("tensor", fn, r, w)
        idf, b_idf = self.identf, self.b_identf
        mt = self.mod_tiles(1, 1)
        rt = A.alloc(64).rearrange("p (k e) -> p k e", k=8); b_rt = P.buf()
        P.dma("sync", rt, self.moe_router.ap().rearrange("(k p) e -> p k e", p=128), writes=[b_rt])
        xts = [(A.alloc(D), P.buf()) for _ in range(2)]
        scrs = [(A.alloc(D, BF16), P.buf(), A.alloc(1), P.buf(), A.alloc(1), P.buf(), A.alloc(D), P.buf(), A.alloc(D, BF16), P.buf()) for _ in range(2)]
        hTs = [(A.alloc(8 * 128, BF16).rearrange("p (k n) -> p k n", k=8), P.buf()) for _ in range(2)]
        hfs = [(A.alloc(D), P.buf()) for _ in range(2)]
        hfT = [(A.alloc(D), P.buf()) for _ in range(2)]
        lg = [(A.alloc(48), P.buf()) for _ in range(2)]
        for t in range(NTILE):
            r0 = t * 128
            ty = self.tile_type(t)
            xt, b_x = xts[t % 2]; scr = scrs[t % 2]; hT, b_hT = hTs[t % 2]
            P.dma("sync", xt, self.X3[r0:r0 + 128, :], writes=[b_x])
            self.norm_mod_transpose(xt, b_x, mt[ty], hT, b_hT, 0, scr)
            P.dma("sync", self.H3T[:, r0:r0 + 128].rearrange("(k p) n -> p k n", p=128), hT, reads=[b_hT])
            h32, b_h32 = scr[6], scr[7]
            Sh, bS = mt[ty][1], mt[ty][3]
            hf, b_hf = hfs[t % 2]; hft, b_hft = hfT[t % 2]; l, b_l = lg[t % 2]
            G_(lambda e, hf=hf, h32=h32, Sh=Sh: e.tensor_tensor(out=hf, in0=h32, in1=Sh, op=ALU.add), [b_h32, bS], [b_hf])
            for half in range(2):
                pt, bp = self.ps()

                def tr(e, pt=pt, hf=hf, half=half):
                    ins = None
                    for k in range(4):
                        kk = half * 4 + k
                        ins = e.transpose(out=pt[:, k * 128:(k + 1) * 128], in_=hf[:, kk * 128:(kk + 1) * 128], identity=idf)
                    return ins
                T_(tr, [b_hf, b_idf], [bp])
                S_(lambda e, hft=hft, pt=pt, half=half: e.copy(out=hft[:, half * 512:(half + 1) * 512], in_=pt[:, :]), [bp], [b_hft])
            pl, bpl = self.ps()

            def lmm(e, pl=pl, hft=hft):
                ins = None
                for k in range(8):
                    ins = e.matmul(pl[:, 0:8], lhsT=hft[:, k * 128:(k + 1) * 128], rhs=rt[:, k, :], start=(k == 0), stop=(k == 7))
                return ins
            T_(lmm, [b_hft, b_rt], [bpl])
            V(lambda e, l=l, pl=pl: e.tensor_copy(out=l[:, 0:8], in_=pl[:, 0:8]), [bpl], [b_l])
            V(lambda e, l=l: e.max(out=l[:, 8:16], in_=l[:, 0:8]), [b_l], [b_l])
            V(lambda e, l=l: e.tensor_tensor(out=l[:, 16:17], in0=l[:, 9:10], in1=l[:, 8:9], op=ALU.subtract), [b_l], [b_l])
            S_(lambda e, l=l: e.activation(out=l[:, 16:17], in_=l[:, 16:17], func=AF.Exp), [b_l], [b_l])
            V(lambda e, l=l: e.tensor_scalar(out=l[:, 16:17], in0=l[:, 16:17], scalar1=1.0, scalar2=None, op0=ALU.add), [b_l], [b_l])
            V(lambda e, l=l: e.reciprocal(out=l[:, 17:18], in_=l[:, 16:17]), [b_l], [b_l])
            V(lambda e, l=l: e.tensor_scalar(out=l[:, 18:19], in0=l[:, 17:18], scalar1=-1.0, scalar2=1.0, op0=ALU.mult, op1=ALU.add), [b_l], [b_l])
            V(lambda e, l=l: e.tensor_scalar(out=l[:, 24:32], in0=l[:, 0:8], scalar1=l[:, 8:9], scalar2=l[:, 17:18], op0=ALU.is_equal, op1=ALU.mult), [b_l], [b_l])
            V(lambda e, l=l: e.tensor_scalar(out=l[:, 32:40], in0=l[:, 0:8], scalar1=l[:, 9:10], scalar2=l[:, 18:19], op0=ALU.is_equal, op1=ALU.mult), [b_l], [b_l])
            V(lambda e, l=l: e.tensor_tensor(out=l[:, 32:40], in0=l[:, 32:40], in1=l[:, 24:32], op=ALU.add), [b_l], [b_l])
            P.dma("sync", self.W8[r0:r0 + 128, :], l[:, 32:40], reads=[b_l])
        A.release(m0)

    def phase_c2(self):
        P, A = self.P, self.A
        m0 = A.mark()
        V = lambda fn, r, w: P.add("vector", fn, r, w)
        S_ = lambda fn, r, w: P.add("scalar", fn, r, w)
        G_ = lambda fn, r, w: P.add("gpsimd", fn, r, w)
        T_ = lambda fn, r, w: P.add("tensor", fn, r, w)
        ED = 3584
        NFC = ED // 512
        GT_ = 2048
        mtg = self.mod_tiles(1, 1, only_gate=True)
        nf = A.alloc(D); b_nf = P.buf()
        P.dma("sync", nf, bass.AP(self.norm_final, 0, [[0, 128], [1, D]]), writes=[b_nf])
        hT = A.alloc(8 * GT_, BF16).rearrange("p (k n) -> p k n", k=8); b_hT = P.buf()
        yacc = [(A.alloc(D), P.buf()) for _ in range(GT_ // 128)]
        wts = A.alloc((GT_ // 128) * 8).rearrange("p (t e) -> p t e", e=8); b_wts = P.buf()
        w1b = [(A.alloc(8 * 512, BF16).rearrange("p (k n) -> p k n", k=8), P.buf()) for _ in range(2)]
        w3b = [(A.alloc(8 * 512, BF16).rearrange("p (k n) -> p k n", k=8), P.buf()) for _ in range(2)]
        w2b = [(A.alloc(4 * D, BF16).rearrange("p (k n) -> p k n", k=4), P.buf()) for _ in range(2)]
        act = A.alloc(4 * GT_, BF16).rearrange("p (k n) -> p k n", k=4); b_act = [P.buf() for _ in range(4)]
        sgs = [(A.alloc(512), P.buf()) for _ in range(2)]
        x3s = [(A.alloc(D), P.buf()) for _ in range(1)]
        outs = [(A.alloc(D), P.buf()) for _ in range(1)]
        st = [(A.alloc(2), P.buf()) for _ in range(2)]
        junk = A.alloc(D, BF16); b_junk = P.buf()
        groups = []
        c = 0
        while c < TT:
            w = min(GT_, TT - c)
            groups.append((c, w))
            c += w
        wi = 0
        for (c0, Wg) in groups:
            ntl = Wg // 128
            P.dma("sync", hT[:, :, 0:Wg], self.H3T[:, c0:c0 + Wg].rearrange("(k p) n -> p k n", p=128), writes=[b_hT])
            P.dma("sync", wts[:, 0:ntl, :], self.W8[c0:c0 + Wg, :].rearrange("(t p) e -> p t e", p=128), writes=[b_wts])
            for tl in range(ntl):
                ya, b_ya = yacc[tl]
                G_(lambda e, ya=ya: e.memset(ya, 0.0), [], [b_ya])
            for ex in range(8):
                for fc in range(NFC):
                    w1, b_w1 = w1b[wi % 2]; w3, b_w3 = w3b[wi % 2]; w2, b_w2 = w2b[wi % 2]
                    wi += 1
                    P.dma("gpsimd", w1, self.moe_w1[ex, :, fc * 512:(fc + 1) * 512].rearrange("(k p) n -> p k n", p=128), writes=[b_w1])
                    P.dma("gpsimd", w3, self.moe_w3[ex, :, fc * 512:(fc + 1) * 512].rearrange("(k p) n -> p k n", p=128), writes=[b_w3])
                    P.dma("gpsimd", w2, self.moe_w2[ex, fc * 512:(fc + 1) * 512, :].rearrange("(k p) n -> p k n", p=128), writes=[b_w2])
                    for ft in range(4):
                        for cc in range(0, Wg, 512):
                            Wc = min(512, Wg - cc)
                            pg, bpg = self.ps(); pu, bpu = self.ps()

                            def mm(e, pg=pg, pu=pu, w1=w1, w3=w3, ft=ft, cc=cc, Wc=Wc):
                                ins = None
                                for k in range(8):
                                    e.matmul(pg[:, 0:Wc], lhsT=w1[:, k, ft * 128:(ft + 1) * 128], rhs=hT[:, k, cc:cc + Wc], start=(k == 0), stop=(k == 7))
                                for k in range(8):
                                    ins = e.matmul(pu[:, 0:Wc], lhsT=w3[:, k, ft * 128:(ft + 1) * 128], rhs=hT[:, k, cc:cc + Wc], start=(k == 0), stop=(k == 7))
                                return ins
                            T_(mm, [b_w1, b_w3, b_hT], [bpg, bpu])
                            sg, b_sg = sgs[(cc // 512) % 2]
                            S_(lambda e, sg=sg, pg=pg, Wc=Wc: e.activation(out=sg[:, 0:Wc], in_=pg[:, 0:Wc], func=AF.Silu), [bpg], [b_sg])
                            V(lambda e, sg=sg, pu=pu, ft=ft, cc=cc, Wc=Wc: e.tensor_tensor(out=act[:, ft, cc:cc + Wc], in0=sg[:, 0:Wc], in1=pu[:, 0:Wc], op=ALU.mult),
                              [b_sg, bpu], [b_act[ft]])
                    for tl in range(ntl):
                        ya, b_ya = yacc[tl]
                        for c2 in range(2):
                            pt, bp = self.ps()

                            def mm2(e, pt=pt, w2=w2, tl=tl, c2=c2):
                                ins = None
                                for ft in range(4):
                                    ins = e.matmul(pt[:, :], lhsT=act[:, ft, tl * 128:(tl + 1) * 128], rhs=w2[:, ft, c2 * 512:(c2 + 1) * 512], start=(ft == 0), stop=(ft == 3))
                                return ins
                            T_(mm2, b_act + [b_w2], [bp])
                            eng = V if (tl + c2) % 2 == 0 else G_
                            if eng is V:
                                V(lambda e, ya=ya, pt=pt, tl=tl, ex=ex, c2=c2: e.scalar_tensor_tensor(out=ya[:, c2 * 512:(c2 + 1) * 512], in0=pt[:, :], scalar=wts[:, tl, ex:ex + 1],
                                                                                                  in1=ya[:, c2 * 512:(c2 + 1) * 512], op0=ALU.mult, op1=ALU.add),
                                  [bp, b_wts, b_ya], [b_ya])
                            else:
                                sg2, b_sg2 = sgs[c2]
                                S_(lambda e, sg2=sg2, pt=pt, tl=tl, ex=ex: e.mul(out=sg2, in_=pt[:, :], mul=wts[:, tl, ex:ex + 1]), [bp, b_wts], [b_sg2])
                                G_(lambda e, ya=ya, sg2=sg2, c2=c2: e.tensor_tensor(out=ya[:, c2 * 512:(c2 + 1) * 512], in0=ya[:, c2 * 512:(c2 + 1) * 512], in1=sg2, op=ALU.add),
                                   [b_sg2, b_ya], [b_ya])
            for tl in range(ntl):
                t = c0 // 128 + tl
                r0 = t * 128
                ty = self.tile_type(t)
                gate = mtg[ty][4]; b_gate = mtg[ty][5]
                ya, b_ya = yacc[tl]
                x3, b_x3 = x3s[0]; o, b_o = outs[0]; s2, b_s2 = st[tl % 2]
                P.dma("sync", x3, self.X3[r0:r0 + 128, :], writes=[b_x3])
                V(lambda e, ya=ya, gate=gate: e.tensor_tensor(out=ya, in0=ya, in1=gate, op=ALU.mult), [b_ya, b_gate], [b_ya])
                G_(lambda e, ya=ya, x3=x3: e.tensor_tensor(out=x3, in0=x3, in1=ya, op=ALU.add), [b_ya, b_x3], [b_x3])
                G_(lambda e, s2=s2: e.memset(s2[:, 0:1], 0.0), [], [b_s2])
                S_(lambda e, x3=x3, s2=s2: e.activation(out=junk, in_=x3, func=AF.Square, accum_out=s2[:, 0:1]), [b_x3, b_s2], [b_junk, b_s2])
                S_(lambda e, s2=s2: e.activation(out=s2[:, 1:2], in_=s2[:, 0:1], func=AF.Sqrt, bias=self.epsc, scale=1.0 / D), [b_s2, self.b_epsc], [b_s2])
                V(lambda e, s2=s2: e.reciprocal(out=s2[:, 1:2], in_=s2[:, 1:2]), [b_s2], [b_s2])
                V(lambda e, o=o, x3=x3, s2=s2: e.scalar_tensor_tensor(out=o, in0=x3, scalar=s2[:, 1:2], in1=nf, op0=ALU.mult, op1=ALU.mult), [b_x3, b_s2, b_nf], [b_o])
                P.dma("sync", self.o_y[r0:r0 + 128, :], o, reads=[b_o])
        A.release(m0)


def host_prep(inp, core):
    f = np.float32
    b = core % 2
    s0 = core * NSAMP
    m = {}
    xall = np.zeros((TT, D), f)
    xall[:SEQ] = inp["x_prompt"][b]
    for s in range(NSAMP):
        xall[SEQ + 64 * s] = inp["x_sample"][s0 + s, 0]
    m["xall"] = xall
    c5 = np.concatenate([inp["c_prompt"][b:b + 1], inp["c_sample"][s0:s0 + NSAMP]], 0)
    m["c5T"] = np.ascontiguousarray(c5.reshape(5, 8, 128).transpose(2, 1, 0).reshape(128, 40))
    for k in ["w_ada", "b_ada", "norm_mix", "norm_ffn"]:
        m[k] = np.ascontiguousarray(inp[k], f)
    m["norm_final"] = np.ascontiguousarray(inp["norm_final"].reshape(1, D), f)
    w = inp["w_in0"]
    qkv = w[:, 0:1536]
    z = w[:, 1536:2048]
    a = w[:, 2048:2052]
    bb = w[:, 2052:2056]
    nq = w[:, 2056:2568]
    kv = w[:, 2568:3336]
    gts = w[:, 3336:3360]
    nqp = np.concatenate([np.concatenate([nq[:, 64 * j:64 * j + 64], nq[:, 64 * (4 + j):64 * (4 + j) + 64]], 1) for j in range(4)], 1)
    kvf = np.concatenate([kv[:, 0:128], kv[:, 128:256], kv[:, 256:384], kv[:, 512:640]], 1)
    m["w_fm0"] = np.ascontiguousarray(np.concatenate([qkv, nqp, kvf], 1), f)
    m["w_tm0"] = np.ascontiguousarray(np.concatenate([z, kv, a, bb, gts], 1), f)
    m["cw0"] = np.ascontiguousarray(inp["gdn_conv_w"].reshape(4, 12, 128).transpose(2, 1, 0).reshape(128, 48), f)
    m["gdn_ab"] = np.concatenate([inp["gdn_a_log"], inp["gdn_dt_bias"]]).reshape(1, 8).astype(f)
    m["sconv0"] = np.ascontiguousarray(inp["state_gdn_conv"][s0:s0 + NSAMP], f)
    m["cwin"] = np.ascontiguousarray(inp["cache_nsa_win"][s0:s0 + NSAMP].reshape(NSAMP, 512, 256), f)
    rv = np.zeros((128, 2), f)
    rv[:, 0] = 1.0
    rv[0, 1] = 1.0
    rv[64, 1] = 1.0
    m["rowvalid"] = rv
    m["ident"] = np.eye(128, dtype=f)
    m["ones"] = np.ones((128, 128), f)
    m["sgdn"] = np.ascontiguousarray(inp["state_gdn"][s0:s0 + NSAMP], f)
    for k_, src_ in [("w_out0", "w_out0"), ("ffn_wg", "ffn_w_gate"), ("ffn_wu", "ffn_w_up"), ("ffn_wd", "ffn_w_down"),
                     ("w_in1", "w_in1"), ("w_out1", "w_out1"), ("lru_wa", "lru_wa"), ("lru_wx", "lru_wx")]:
        m[k_] = np.ascontiguousarray(inp[src_], f)
    m["cache2d"] = np.ascontiguousarray(inp["cache_nsa_kv"].reshape(NPOOL * 128, 512), f)
    m["ptab"] = np.ascontiguousarray(inp["page_table"][s0:s0 + NSAMP], np.int32)
    m["iotap"] = np.arange(128, dtype=f).reshape(128, 1)
    hw = np.zeros((128, 256), f)
    hw[:64, 126] = 1.0 / 64
    hw[64:, 127] = 1.0 / 64
    m["hwide"] = hw
    m["slconv"] = np.ascontiguousarray(inp["state_lru_conv"][s0:s0 + NSAMP], f)
    m["slru"] = np.ascontiguousarray(inp["state_lru"][s0:s0 + NSAMP], f)
    for k_ in ["moe_router", "moe_w1", "moe_w3", "moe_w2"]:
        m[k_] = np.ascontiguousarray(inp[k_], f)
    lv = np.stack([inp["lru_conv_w"][0], inp["lru_conv_w"][1], inp["lru_conv_w"][2], inp["lru_conv_w"][3],
                   inp["lru_conv_b"], inp["lru_ba"], inp["lru_bx"], inp["lru_lambda"]], 0).astype(f)
    m["lru_vec"] = np.ascontiguousarray(lv.reshape(8, 8, 128).transpose(2, 1, 0).reshape(128, 64))
    kk = np.arange(128)[:, None]
    qq = np.arange(128)[None, :]
    def bidx(dist, extra=None):
        b = t5_bucket_np(dist).astype(np.float32)
        bad = dist < 0
        if extra is not None:
            bad = bad | extra
        return np.where(bad, 32.0, b).astype(np.float32)
    m["idx_sel"] = np.stack([bidx(128 * dd + qq - kk).reshape(-1) for dd in range(14)])
    m["idx_win"] = np.stack([bidx(128 * dd + qq - kk, (128 * dd + qq - kk) >= 512).reshape(-1) for dd in range(5)])
    qc = np.arange(128)[:, None]
    rr = np.arange(256)[None, :]
    m["idx_cmp"] = bidx(qc - 63 - 64 * (rr - 128)).reshape(1, -1)
    m["relb33"] = np.concatenate([inp["rel_bias"].astype(f), np.full((1, 8), -30000.0, f)], 0)
    m["iota33"] = np.arange(33, dtype=f).reshape(33, 1)
    m["wide"] = (np.arange(128 * 65)[None, :] // 64 == np.arange(128)[:, None]).astype(f)
    hi = (qc >= 64).astype(np.int64)
    rel = rr - 128 - hi
    vm = (rel <= 0).astype(f)
    fa = np.where((rel == 0) | (rel == -1), 1.0e4, np.where(rel <= 0, 0.0, -1.0)).astype(f)
    m["vmfa"] = np.concatenate([vm, fa], 1)
    m["gnw"] = np.ascontiguousarray(inp["gdn_norm_w"].reshape(1, 128), f)
    pi = np.arange(64)[:, None]
    fi = np.arange(64)[None, :]
    m["umask"] = np.concatenate([(pi <= fi), (pi > fi), (pi >= fi)], 1).astype(f)
    return m


_CACHE = {}


def get_builder(dbg=(), phases=("p0", "a1", "gdn", "nsa", "b1", "b2", "c")):
    key = (tuple(dbg), tuple(phases))
    if key not in _CACHE:
        bld = Builder(dbg)
        bld.build(phases)
        _CACHE[key] = bld
    return _CACHE[key]


def kernel(**inputs):
    inp = {k: np.asarray(v) for k, v in inputs.items()}
    bld = get_builder()
    in_maps = []
    for c in range(8):
        m = host_prep(inp, c)
        in_maps.append({k: m[k] for k in bld.inputs})
    res = run_bass_kernel_spmd(bld.nc, in_maps, core_ids=list(range(8)))
    r = res.results
    f = np.float32
    B = 2
    y_prompt = np.stack([r[b]["o_y"][:SEQ] for b in range(B)])
    y_sample = np.concatenate([r[c]["o_y"][SEQ::64][:NSAMP].reshape(NSAMP, 1, D) for c in range(8)])
    p_kv = np.stack([r[b]["o_pkv"].reshape(SEQ, 4, 2, 64) for b in range(B)])
    p_win = np.stack([r[b]["o_pwin"].reshape(512, 2, 2, 64) for b in range(B)])
    p_gdn = np.stack([r[b]["o_pgdn"] for b in range(B)])
    p_gconv = np.stack([r[b]["o_pgconv"] for b in range(B)])
    p_lru = np.stack([r[b]["o_plru"][0] for b in range(B)])
    p_lconv = np.stack([r[b]["o_plconv"] for b in range(B)])
    s_kv = np.concatenate([r[c]["o_skv"].reshape(NSAMP, 1, 4, 2, 64) for c in range(8)])
    s_win = np.concatenate([r[c]["o_swin"].reshape(NSAMP, 512, 2, 2, 64) for c in range(8)])
    s_gdn = np.concatenate([r[c]["o_sgdn"] for c in range(8)])
    s_gconv = np.concatenate([r[c]["o_sgconv"] for c in range(8)])
    s_lru = np.concatenate([r[c]["o_slru"] for c in range(8)])
    s_lconv = np.concatenate([r[c]["o_slconv"] for c in range(8)])
    return (y_prompt, y_sample, p_kv, p_win, p_gdn, p_gconv, p_lru, p_lconv,
            s_kv, s_win, s_gdn, s_gconv, s_lru, s_lconv)
```
